# Optimizing a Trainium2 kernel written in Bass

```python
import math
import jax
import jax.numpy as jnp
from jax import lax
import numpy as np

D_MODEL = 1024
BATCH = 8
SEQ = 4096
DEPTH = 4

CTX_LEN = 256
GRID_W = 64
N_MIXERS = 3
CHUNK = 128
N_RET = (DEPTH + 2) // 3
N_MLSTM = (DEPTH + 1) // 3
N_RWKV = DEPTH // 3

RET_HEADS = 4
RET_DK = D_MODEL // RET_HEADS
RET_QK = RET_HEADS * RET_DK
RET_V = 2 * D_MODEL
RET_DV = RET_V // RET_HEADS
RET_IN = 2 * RET_QK + 2 * RET_V
ROPE_BASE = 10000.0

MLSTM_INNER = 2 * D_MODEL
MLSTM_HEADS = 4
MLSTM_DH = MLSTM_INNER // MLSTM_HEADS
QKV_BLOCK = 4
N_QKV_BLOCKS = MLSTM_INNER // QKV_BLOCK
MLSTM_CONV = 3

RWKV_N = 64
RWKV_HEADS = D_MODEL // RWKV_N
RWKV_DECAY_LORA = 64
RWKV_A_LORA = 64

DEEPNORM_ALPHA = (2.0 * DEPTH) ** 0.25
DEEPNORM_BETA = (8.0 * DEPTH) ** -0.25
LN_EPS = 1e-5
HEAD_NORM_EPS = 1e-6
RWKV_GN_EPS = 64e-5

kernel_name = "hybrid_retnet_mlstm_rwkv7_deepnorm_dit"


def _layer_norm(x, g, b):
    xf = x.astype(jnp.float32)
    mu = xf.mean(-1, keepdims=True)
    var = jnp.mean(jnp.square(xf - mu), -1, keepdims=True)
    return ((xf - mu) * lax.rsqrt(var + LN_EPS) * g.astype(jnp.float32) + b.astype(jnp.float32)).astype(x.dtype)


def _chunks(t):
    B, H, T = t.shape[:3]
    return jnp.moveaxis(t.reshape(B, H, T // CHUNK, CHUNK, *t.shape[3:]), 2, 0)


def _unchunks(t):
    t = jnp.moveaxis(t, 0, 2)
    return t.reshape(t.shape[0], t.shape[1], -1, *t.shape[4:])


def _rope_2d(x, rows, cols):
    half = x.shape[-1] // 2
    nf = half // 2
    inv = ROPE_BASE ** (-jnp.arange(nf, dtype=jnp.float32) / nf)

    def rot(xa, pos):
        ang = pos[:, None] * inv[None, :]
        cos, sin = jnp.cos(ang), jnp.sin(ang)
        x1, x2 = xa[..., :nf], xa[..., nf:]
        return jnp.concatenate([x1 * cos - x2 * sin, x1 * sin + x2 * cos], -1)

    return jnp.concatenate([rot(x[..., :half], rows), rot(x[..., half:], cols)], -1)


def _retention_scan(q, k, v, log_gamma, state0):
    idx = jnp.arange(CHUNK, dtype=jnp.float32)
    lg = log_gamma[:, None]
    diff = idx[:, None] - idx[None, :]
    decay_mask = jnp.where(diff >= 0, jnp.exp(diff * lg[:, :, None]), 0.0)
    q_decay = jnp.exp((idx + 1.0) * lg)[:, :, None]
    k_decay = jnp.exp((CHUNK - 1.0 - idx) * lg)[:, :, None]
    chunk_decay = jnp.exp(CHUNK * log_gamma)[:, None, None]

    def step(S, inp):
        qc, kc, vc = inp
        s = jnp.einsum('bhid,bhjd->bhij', qc, kc) * decay_mask
        o = jnp.einsum('bhij,bhjv->bhiv', s, vc) + jnp.einsum('bhid,bhdv->bhiv', qc * q_decay, S)
        S = S * chunk_decay + jnp.einsum('bhjd,bhjv->bhdv', kc * k_decay, vc)
        return S, o

    S, o = lax.scan(step, state0, (_chunks(q), _chunks(k), _chunks(v)))
    return _unchunks(o), S


def _retention_mixer(h, hc, w_in, decay_logit, w_out, ctx_out):
    f32 = jnp.float32
    B, T, _ = h.shape

    def project(u):
        p = (u @ w_in).astype(f32)
        q, k, v, g = jnp.split(p, [RET_QK, 2 * RET_QK, 2 * RET_QK + RET_V], axis=-1)
        heads = lambda t, d: t.reshape(t.shape[0], t.shape[1], RET_HEADS, d).transpose(0, 2, 1, 3)
        return heads(q, RET_DK), heads(k, RET_DK) * RET_DK ** -0.5, heads(v, RET_DV), g

    q, k, v, g = project(h)
    qc, kc, vc, gc = project(hc)
    t = jnp.arange(T)
    rows = (t // GRID_W).astype(f32)
    cols = (t % GRID_W).astype(f32)
    q = _rope_2d(q, rows, cols)
    k = _rope_2d(k, rows, cols)
    log_gamma = jax.nn.log_sigmoid(decay_logit.astype(f32))
    zero = jnp.zeros((B, RET_HEADS, RET_DK, RET_DV), f32)

    def direction(d, rev):
        f = (lambda a: jnp.flip(a, axis=2)) if rev else (lambda a: a)
        oc_d, state = _retention_scan(f(qc), f(kc), f(vc), log_gamma[d], zero)
        o_d, _ = _retention_scan(f(q), f(k), f(v), log_gamma[d], state)
        return f(o_d), f(oc_d)

    o_f, oc_f = direction(0, False)
    o_b, oc_b = direction(1, True)

    def finish(o, gate):
        o = o * lax.rsqrt(jnp.mean(o * o, -1, keepdims=True) + HEAD_NORM_EPS)
        o = o.transpose(0, 2, 1, 3).reshape(o.shape[0], o.shape[2], RET_V)
        return (jax.nn.silu(gate) * o).astype(h.dtype) @ w_out

    y = finish(o_f + o_b, g)
    yc = finish(oc_f + oc_b, gc) if ctx_out else None
    return y, yc


def _centred_dwconv(u, w, b):
    K = w.shape[0]
    p = K // 2
    T = u.shape[1]
    up = jnp.pad(u, ((0, 0), (p, p), (0, 0)))
    out = b + up[:, 0:T] * w[0]
    for j in range(1, K):
        out = out + up[:, j:j + T] * w[j]
    return out


def _headwise(u, w):
    B, T, _ = u.shape
    return jnp.einsum('btnc,ncd->btnd', u.reshape(B, T, N_QKV_BLOCKS, QKV_BLOCK), w).reshape(B, T, MLSTM_INNER)


def _mlstm_scan(q, k, v, i_pre, logf, state0):
    tril = jnp.tril(jnp.ones((CHUNK, CHUNK), dtype=bool))

    def step(carry, inp):
        C, n, m = carry
        qc, kc, vc, ic, fc = inp
        b = jnp.cumsum(fc, axis=-1)
        a = b + m[..., None]
        dlog = jnp.where(tril, b[..., :, None] - b[..., None, :] + ic[..., None, :], -jnp.inf)
        m_t = jnp.maximum(a, dlog.max(-1))
        s = jnp.einsum('bhid,bhjd->bhij', qc, kc) * jnp.exp(dlog - m_t[..., None])
        inter = jnp.exp(a - m_t)
        num = jnp.einsum('bhij,bhjv->bhiv', s, vc) + inter[..., None] * jnp.einsum('bhid,bhdv->bhiv', qc, C)
        den = s.sum(-1) + inter * jnp.einsum('bhid,bhd->bhi', qc, n)
        hout = num / jnp.maximum(jnp.abs(den), jnp.exp(-m_t))[..., None]
        bl = b[..., -1]
        wlog = bl[..., None] - b + ic
        m_new = jnp.maximum(bl + m, wlog.max(-1))
        wk = kc * jnp.exp(wlog - m_new[..., None])[..., None]
        decay = jnp.exp(bl + m - m_new)
        C = decay[..., None, None] * C + jnp.einsum('bhjd,bhjv->bhdv', wk, vc)
        n = decay[..., None] * n + wk.sum(2)
        return (C, n, m_new), hout

    carry, hs = lax.scan(step, state0, (_chunks(q), _chunks(k), _chunks(v), _chunks(i_pre), _chunks(logf)))
    return _unchunks(hs), carry


def _mlstm_mixer(h, hc, w_in, conv_w, conv_b, w_qkv, gate_w, gate_b, skip, gn_g, w_out, ctx_out):
    f32 = jnp.float32
    B = h.shape[0]
    heads = lambda t: t.reshape(t.shape[0], t.shape[1], MLSTM_HEADS, MLSTM_DH).transpose(0, 2, 1, 3).astype(f32)

    def prep(u):
        xm, z = jnp.split(u @ w_in, 2, axis=-1)
        xconv = jax.nn.silu(_centred_dwconv(xm, conv_w, conv_b))
        q = _headwise(xconv, w_qkv[0])
        k = _headwise(xconv, w_qkv[1])
        v = _headwise(xm, w_qkv[2])
        qkv = jnp.concatenate([q, k, v], axis=-1)
        gates = [jnp.moveaxis((qkv @ gate_w[d] + gate_b[d]).astype(f32), -1, 1) for d in range(2)]
        ig = [gd[:, :MLSTM_HEADS] for gd in gates]
        lf = [jax.nn.log_sigmoid(gd[:, MLSTM_HEADS:]) for gd in gates]
        return heads(q), heads(k) * MLSTM_DH ** -0.5, heads(v), ig, lf, xconv, z

    q, k, v, ig, lf, xconv, z = prep(h)
    qc, kc, vc, igc, lfc, xconvc, zc = prep(hc)
    zero = (jnp.zeros((B, MLSTM_HEADS, MLSTM_DH, MLSTM_DH), f32),
            jnp.zeros((B, MLSTM_HEADS, MLSTM_DH), f32),
            jnp.zeros((B, MLSTM_HEADS), f32))

    def direction(d, rev):
        f = (lambda a: jnp.flip(a, axis=2)) if rev else (lambda a: a)
        oc_d, state = _mlstm_scan(f(qc), f(kc), f(vc), f(igc[d]), f(lfc[d]), zero)
        o_d, _ = _mlstm_scan(f(q), f(k), f(v), f(ig[d]), f(lf[d]), state)
        return f(o_d), f(oc_d)

    o_f, oc_f = direction(0, False)
    o_b, oc_b = direction(1, True)

    def finish(o, xcv, zz):
        Bn, _, Tn, _ = o.shape
        mu = o.mean(-1, keepdims=True)
        var = jnp.mean(jnp.square(o - mu), -1, keepdims=True)
        o = ((o - mu) * lax.rsqrt(var + LN_EPS)).transpose(0, 2, 1, 3).reshape(Bn, Tn, MLSTM_INNER) * gn_g
        o = (o + skip * xcv.astype(f32)) * jax.nn.silu(zz.astype(f32))
        return o.astype(h.dtype) @ w_out

    y = finish(o_f + o_b, xconv, z)
    yc = finish(oc_f + oc_b, xconvc, zc) if ctx_out else None
    return y, yc


def _qshift_grid(u):
    B, T, D = u.shape
    g = u.reshape(B, T // GRID_W, GRID_W, D)
    q = D // 4
    left = jnp.pad(g[:, :, :-1, :q], ((0, 0), (0, 0), (1, 0), (0, 0)))
    right = jnp.pad(g[:, :, 1:, q:2 * q], ((0, 0), (0, 0), (0, 1), (0, 0)))
    up = jnp.pad(g[:, :-1, :, 2 * q:3 * q], ((0, 0), (1, 0), (0, 0), (0, 0)))
    down = jnp.pad(g[:, 1:, :, 3 * q:], ((0, 0), (0, 1), (0, 0), (0, 0)))
    return jnp.concatenate([left, right, up, down], -1).reshape(B, T, D)


def _shift_seq(u):
    hd = u.shape[-1] // 2
    prev = jnp.pad(u[:, :-1, :hd], ((0, 0), (1, 0), (0, 0)))
    nxt = jnp.pad(u[:, 1:, hd:], ((0, 0), (0, 1), (0, 0)))
    return jnp.concatenate([prev, nxt], -1)


def _rwkv_scan(r, w, k, v, a, b, state0):
    xs = tuple(jnp.moveaxis(t, 1, 0) for t in (r, w, k, v, a, b))

    def step(S, inp):
        rt, wt, kt, vt, at, bt = inp
        sa = jnp.einsum('bhij,bhj->bhi', S, at)
        S = S * wt[:, :, None, :] + sa[..., None] * bt[:, :, None, :] + vt[..., None] * kt[:, :, None, :]
        return S, jnp.einsum('bhij,bhj->bhi', S, rt)

    S, ys = lax.scan(step, state0, xs)
    return jnp.moveaxis(ys, 0, 1), S


def _rwkv7_mixer(h, hc, mix, w_rkvg, w0, w1, w2, a0, a1, a2, k_k, k_a, r_k, gn_g, gn_b, w_out, ctx_out):
    f32 = jnp.float32
    B = h.shape[0]
    heads = lambda t: t.reshape(t.shape[0], t.shape[1], RWKV_HEADS, RWKV_N).astype(f32)

    def prep(u, shifted):
        xx = shifted - u
        xr, xw, xk, xv, xa, xg = (u + xx * mix[j] for j in range(6))
        r, k, v, g = jnp.einsum('nbtd,nde->nbte', jnp.stack([xr, xk, xv, xg]), w_rkvg)
        kk = heads(k * k_k)
        kk = kk / jnp.maximum(jnp.sqrt(jnp.sum(kk * kk, -1, keepdims=True)), 1e-12)
        per_dir = []
        for d in range(2):
            wlog = -jax.nn.softplus(-(w0[d] + jnp.tanh(xw @ w1[d]) @ w2[d]).astype(f32)) - 0.5
            ad = jax.nn.sigmoid((a0[d] + (xa @ a1[d]) @ a2[d]).astype(f32))
            kd = k.astype(f32) * (1.0 + (ad - 1.0) * k_a.astype(f32))
            per_dir.append((heads(jnp.exp(-jnp.exp(wlog))), heads(kd), -kk, kk * heads(ad)))
        return heads(r), heads(v), g, per_dir

    r, v, g, dirs = prep(h, _qshift_grid(h))
    rc, vc, gc, dirs_c = prep(hc, _shift_seq(hc))
    zero = jnp.zeros((B, RWKV_HEADS, RWKV_N, RWKV_N), f32)

    def direction(d, rev):
        f = (lambda t: jnp.flip(t, axis=1)) if rev else (lambda t: t)
        wc_, kc_, ac_, bc_ = dirs_c[d]
        yc_d, state = _rwkv_scan(f(rc), f(wc_), f(kc_), f(vc), f(ac_), f(bc_), zero)
        w_, k_, a_, b_ = dirs[d]
        y_d, _ = _rwkv_scan(f(r), f(w_), f(k_), f(v), f(a_), f(b_), state)
        return f(y_d), f(yc_d)

    y_f, yc_f = direction(0, False)
    y_b, yc_b = direction(1, True)

    def finish(y, r_, v_, k_fwd, k_bwd, gate):
        Bn, Tn = y.shape[:2]
        mu = y.mean(-1, keepdims=True)
        var = jnp.mean(jnp.square(y - mu), -1, keepdims=True)
        o = ((y - mu) * lax.rsqrt(var + RWKV_GN_EPS)).reshape(Bn, Tn, D_MODEL) * gn_g + gn_b
        bonus = ((r_ * k_fwd * r_k).sum(-1, keepdims=True) * v_
                 + (r_ * k_bwd * r_k).sum(-1, keepdims=True) * v_).reshape(Bn, Tn, D_MODEL)
        o = (o + bonus) * jax.nn.silu(gate.astype(f32))
        return o.astype(h.dtype) @ w_out

    y = finish(y_f + y_b, r, v, dirs[0][1], dirs[1][1], g)
    yc = finish(yc_f + yc_b, rc, vc, dirs_c[0][1], dirs_c[1][1], gc) if ctx_out else None
    return y, yc


def setup_inputs(seed: int = 0) -> dict:
    key = jax.random.key(seed)
    ks = iter(jax.random.split(key, 48))
    nrm = lambda shape, s: jax.random.normal(next(ks), shape, jnp.float32) * s
    D, I, H = D_MODEL, MLSTM_INNER, MLSTM_HEADS

    gamma0 = 1.0 - 2.0 ** (-5.0 - np.arange(RET_HEADS))
    ret_decay0 = jnp.asarray(np.log(gamma0 / (1.0 - gamma0)), jnp.float32)

    return {
        "x": nrm((BATCH, SEQ, D), 1.0),
        "c": nrm((BATCH, D), 1.0),
        "ctx": nrm((BATCH, CTX_LEN, D), 1.0),
        "c_ctx": nrm((D,), 1.0),
        "ada_w": nrm((DEPTH, D, 3 * D), D ** -0.5),
        "ada_b": nrm((DEPTH, 3 * D), 0.02),
        "ln_g": 1.0 + nrm((DEPTH, D), 0.02),
        "ln_b": nrm((DEPTH, D), 0.02),
        "ret_w_in": nrm((N_RET, D, RET_IN), D ** -0.5),
        "ret_decay": ret_decay0 + nrm((N_RET, 2, RET_HEADS), 0.05),
        "ret_w_out": nrm((N_RET, RET_V, D), RET_V ** -0.5 * DEEPNORM_BETA),
        "ml_w_in": nrm((N_MLSTM, D, 2 * I), D ** -0.5),
        "ml_conv_w": nrm((N_MLSTM, MLSTM_CONV, I), MLSTM_CONV ** -0.5),
        "ml_conv_b": nrm((N_MLSTM, I), 0.02),
        "ml_w_qkv": nrm((N_MLSTM, 3, N_QKV_BLOCKS, QKV_BLOCK, QKV_BLOCK), QKV_BLOCK ** -0.5),
        "ml_gate_w": nrm((N_MLSTM, 2, 3 * I, 2 * H), 0.1 * (3 * I) ** -0.5),
        "ml_gate_b": jnp.concatenate([nrm((N_MLSTM, 2, H), 0.1),
                                      jnp.linspace(3.0, 6.0, H) + nrm((N_MLSTM, 2, H), 0.05)], axis=-1),
        "ml_skip": 1.0 + nrm((N_MLSTM, I), 0.02),
        "ml_gn_g": 1.0 + nrm((N_MLSTM, I), 0.02),
        "ml_w_out": nrm((N_MLSTM, I, D), I ** -0.5 * DEEPNORM_BETA),
        "rk_mix": jax.random.uniform(next(ks), (N_RWKV, 6, D), jnp.float32, 0.2, 0.8),
        "rk_w_rkvg": nrm((N_RWKV, 4, D, D), D ** -0.5),
        "rk_w0": jnp.linspace(-6.0, -1.0, D) + nrm((N_RWKV, 2, D), 0.1),
        "rk_w1": nrm((N_RWKV, 2, D, RWKV_DECAY_LORA), 0.1 * D ** -0.5),
        "rk_w2": nrm((N_RWKV, 2, RWKV_DECAY_LORA, D), 0.1 * RWKV_DECAY_LORA ** -0.5),
        "rk_a0": nrm((N_RWKV, 2, D), 0.1),
        "rk_a1": nrm((N_RWKV, 2, D, RWKV_A_LORA), 0.1 * D ** -0.5),
        "rk_a2": nrm((N_RWKV, 2, RWKV_A_LORA, D), 0.1 * RWKV_A_LORA ** -0.5),
        "rk_k_k": 0.85 + nrm((N_RWKV, D), 0.02),
        "rk_k_a": 1.0 + nrm((N_RWKV, D), 0.02),
        "rk_r_k": nrm((N_RWKV, RWKV_HEADS, RWKV_N), 0.1),
        "rk_gn_g": 1.0 + nrm((N_RWKV, D), 0.02),
        "rk_gn_b": nrm((N_RWKV, D), 0.02),
        "rk_w_out": nrm((N_RWKV, D, D), D ** -0.5 * DEEPNORM_BETA),
    }


def reference(x, c, ctx, c_ctx, ada_w, ada_b, ln_g, ln_b,
              ret_w_in, ret_decay, ret_w_out,
              ml_w_in, ml_conv_w, ml_conv_b, ml_w_qkv, ml_gate_w, ml_gate_b, ml_skip, ml_gn_g, ml_w_out,
              rk_mix, rk_w_rkvg, rk_w0, rk_w1, rk_w2, rk_a0, rk_a1, rk_a2, rk_k_k, rk_k_a, rk_r_k,
              rk_gn_g, rk_gn_b, rk_w_out):
    sc = jax.nn.silu(c)
    scc = jax.nn.silu(c_ctx)
    xc = ctx
    for i in range(DEPTH):
        last = i == DEPTH - 1
        mod = sc @ ada_w[i] + ada_b[i]
        modc = scc @ ada_w[i] + ada_b[i]
        shift, scale, gate = jnp.split(mod[:, None, :], 3, axis=-1)
        shift_c, scale_c, gate_c = jnp.split(modc, 3)
        h = x * (1.0 + scale) + shift
        hc = xc * (1.0 + scale_c) + shift_c
        kind, j = i % N_MIXERS, i // N_MIXERS
        if kind == 0:
            y, yc = _retention_mixer(h, hc, ret_w_in[j], ret_decay[j], ret_w_out[j], not last)
        elif kind == 1:
            y, yc = _mlstm_mixer(h, hc, ml_w_in[j], ml_conv_w[j], ml_conv_b[j], ml_w_qkv[j], ml_gate_w[j],
                                 ml_gate_b[j], ml_skip[j], ml_gn_g[j], ml_w_out[j], not last)
        else:
            y, yc = _rwkv7_mixer(h, hc, rk_mix[j], rk_w_rkvg[j], rk_w0[j], rk_w1[j], rk_w2[j], rk_a0[j],
                                 rk_a1[j], rk_a2[j], rk_k_k[j], rk_k_a[j], rk_r_k[j], rk_gn_g[j], rk_gn_b[j],
                                 rk_w_out[j], not last)
        x = _layer_norm(DEEPNORM_ALPHA * x + gate * y, ln_g[i], ln_b[i])
        if not last:
            xc = _layer_norm(DEEPNORM_ALPHA * xc + gate_c * yc, ln_g[i], ln_b[i])
    return x
```

```python
import contextlib
import numpy as np
import concourse.bass as bass
import concourse.mybir as mybir
from concourse.bass_utils import run_bass_kernel_spmd

F32 = mybir.dt.float32
BF16 = mybir.dt.bfloat16
AF = mybir.ActivationFunctionType
ALU = mybir.AluOpType

D = 1024
SEQ = 4096
CTX = 256
NT = SEQ + CTX
NCH = NT // 128
DEPTH = 4
ALPHA = (2.0 * DEPTH) ** 0.25
LN_EPS = 1e-5

RK_DBG = 0


class StopBuild(Exception):
    pass


def ck(n):
    if RK_DBG == 100 + n:
        raise StopBuild()
SEM_CHUNK = 4000
N_DMA_SEMS = 40


class Buf:
    __slots__ = ("name", "lw", "rs", "psum")

    def __init__(self, name, psum=False):
        self.name = name
        self.lw = None
        self.rs = []
        self.psum = psum


class Op:
    __slots__ = ("stream", "fn", "deps", "is_dma", "seq", "signals", "sigidx", "clock",
                 "dsem", "dval", "dprev")


class Prog:
    def __init__(self, nc):
        self.nc = nc
        self.streams = {"pe": [], "dve": [], "act": [], "pool": [], "sp": []}
        self.known = {s: {} for s in self.streams}
        self.known_dma = {s: {} for s in self.streams}
        self.dma_cnt = [0] * N_DMA_SEMS
        self.dma_last = [None] * N_DMA_SEMS
        self.dma_rr = {"sp": 0, "pool": 0}
        self.nsig = {s: 0 for s in self.streams}
        self.last_compute = {s: None for s in self.streams}

    def _finish_deps(self, stream, deps, is_dma):
        kn = self.known[stream]
        kd = self.known_dma[stream]
        best = {}
        final = []
        for p in deps:
            if p.is_dma:
                if kd.get(p.dsem, 0) >= p.dval:
                    continue
                final.append(p)
            else:
                if p.stream == stream and stream == "pe" and not is_dma:
                    continue
                if kn.get(p.stream, -1) >= p.seq:
                    continue
                if p.stream not in best or best[p.stream].seq < p.seq:
                    best[p.stream] = p
        final.extend(best.values())
        for p in final:
            if p.is_dma:
                kd[p.dsem] = max(kd.get(p.dsem, 0), p.dval)
            else:
                p.signals = True
                kn[p.stream] = max(kn.get(p.stream, -1), p.seq)
            for k, v in p.clock[0].items():
                if kn.get(k, -1) < v:
                    kn[k] = v
            for k, v in p.clock[1].items():
                if kd.get(k, 0) < v:
                    kd[k] = v
        return final

    def _add(self, stream, fn, reads, writes, is_dma):
        op = Op()
        op.stream = stream
        op.fn = fn
        op.is_dma = is_dma
        op.signals = False
        op.sigidx = None
        st = self.streams[stream]
        op.seq = len(st)
        deps = []
        seen = set()

        def need(p):
            if p is None or id(p) in seen:
                return
            seen.add(id(p))
            deps.append(p)

        for b in reads:
            need(b.lw)
            if b.psum:
                for r in b.rs:
                    if r.stream != stream:
                        need(r)
        for b in writes:
            need(b.lw)
            for r in b.rs:
                need(r)
        op.deps = self._finish_deps(stream, deps, is_dma)
        op.dprev = 0
        if is_dma:
            kd = self.known_dma[stream]
            half = N_DMA_SEMS // 2
            base = 0 if stream == "sp" else half
            s = base + self.dma_rr[stream]
            self.dma_rr[stream] = (self.dma_rr[stream] + 1) % half
            self.dma_cnt[s] += 1
            op.dsem = s
            op.dval = 16 * self.dma_cnt[s]
            op.dprev = op.dval - 16
            if kd.get(s, 0) < op.dprev:
                kd[s] = op.dprev
            self.dma_last[s] = op
        else:
            self.last_compute[stream] = op
        op.clock = (dict(self.known[stream]), dict(self.known_dma[stream]))
        st.append(op)
        for b in reads:
            b.rs.append(op)
        for b in writes:
            b.lw = op
            b.rs = []
        return op

    def pe(self, fn, reads=(), writes=()):
        return self._add("pe", fn, reads, writes, False)

    def dve(self, fn, reads=(), writes=()):
        return self._add("dve", fn, reads, writes, False)

    def act(self, fn, reads=(), writes=()):
        return self._add("act", fn, reads, writes, False)

    def pool(self, fn, reads=(), writes=()):
        return self._add("pool", fn, reads, writes, False)

    def on(self, eng, fn, reads=(), writes=()):
        return self._add(eng, fn, reads, writes, False)

    def dma(self, out, in_, reads=(), writes=(), q="sp", **kw):
        return self._add(q, lambda e: e.dma_start(out=out, in_=in_, **kw), reads, writes, True)

    def barrier(self):
        lasts = [op for op in self.last_compute.values() if op is not None]
        lasts += [op for op in self.dma_last if op is not None]
        for s in self.streams:
            op = Op()
            op.stream = s
            op.fn = None
            op.is_dma = False
            op.signals = False
            op.sigidx = None
            op.dprev = 0
            op.seq = len(self.streams[s])
            op.deps = self._finish_deps(s, list(lasts), False)
            op.clock = (dict(self.known[s]), dict(self.known_dma[s]))
            self.streams[s].append(op)

    def emit(self, final_waits=()):
        nc = self.nc
        for s, st in self.streams.items():
            n = 0
            for op in st:
                if not op.is_dma and op.signals:
                    op.sigidx = n
                    n += 1
            self.nsig[s] = n
        with contextlib.ExitStack() as es:
            csems = {}
            for s in self.streams:
                k = (self.nsig[s] + SEM_CHUNK - 1) // SEM_CHUNK
                csems[s] = [es.enter_context(nc.semaphore(f"c_{s}_{i}")) for i in range(k)]
            dsems = [es.enter_context(nc.semaphore(f"d_{i}")) for i in range(N_DMA_SEMS)]
            block = es.enter_context(nc.Block())

            def run(stream, eng):
                for op in self.streams[stream]:
                    for p in op.deps:
                        if p.is_dma:
                            eng.wait_ge(dsems[p.dsem], p.dval)
                        else:
                            eng.wait_ge(csems[p.stream][p.sigidx // SEM_CHUNK],
                                        p.sigidx % SEM_CHUNK + 1)
                    if op.fn is None:
                        continue
                    if op.is_dma and op.dprev > 0:
                        eng.wait_ge(dsems[op.dsem], op.dprev)
                    ins = op.fn(eng)
                    if op.is_dma:
                        ins.then_inc(dsems[op.dsem], 16)
                    elif op.signals:
                        ins.then_inc(csems[stream][op.sigidx // SEM_CHUNK], 1)
                if stream == "sp":
                    for p in final_waits:
                        eng.wait_ge(dsems[p.dsem], p.dval)

            @block.tensor
            def _(e):
                run("pe", e)

            @block.vector
            def _(e):
                run("dve", e)

            @block.scalar
            def _(e):
                run("act", e)

            @block.gpsimd
            def _(e):
                run("pool", e)

            @block.sync
            def _(e):
                run("sp", e)


class T:
    __slots__ = ("ap", "b")

    def __init__(self, ap, name):
        self.ap = ap
        self.b = Buf(name)


class Arena:
    def __init__(self, ap):
        self.a = ap
        self.off = 0
        self.cap = ap.shape[1]

    def f32(self, name, n):
        v = self.a[:, self.off:self.off + n]
        self.off += n
        assert self.off <= self.cap, (name, self.off, self.cap)
        return T(v, name)

    def bf16(self, name, n):
        w = (n + 1) // 2
        v = self.a[:, self.off:self.off + w].bitcast(BF16)[:, 0:n]
        self.off += w
        assert self.off <= self.cap, (name, self.off, self.cap)
        return T(v, name)


class K:
    def __init__(self, P):
        self.P = P

    def mm(self, O, o, A, a, B, b, start, stop):
        rd = [A, B] if not isinstance(A, (list, tuple)) else list(A) + [B]
        self.P.pe(lambda e: e.matmul(o, lhsT=a, rhs=b, start=start, stop=stop), reads=rd, writes=[O])

    def tr(self, O, o, A, a, I, i):
        self.P.pe(lambda e: e.transpose(out=o, in_=a, identity=i), reads=[A, I], writes=[O])

    def act(self, O, o, A, a, func, bias=0.0, scale=1.0, accum=None, extra=(), eng="act", wextra=()):
        if accum is None:
            self.P.act(lambda e: e.activation(out=o, in_=a, func=func, bias=bias, scale=scale),
                       reads=[A] + list(extra), writes=[O] + list(wextra))
        else:
            self.P.act(lambda e: e.activation(out=o, in_=a, func=func, bias=bias, scale=scale, accum_out=accum),
                       reads=[A] + list(extra), writes=[O] + list(wextra))

    def cp(self, eng, O, o, A, a):
        if eng == "act":
            self.P.act(lambda e: e.copy(out=o, in_=a), reads=[A], writes=[O])
        else:
            self.P.on(eng, lambda e: e.tensor_copy(out=o, in_=a), reads=[A], writes=[O])

    def tt(self, eng, O, o, A, a, B, b, op):
        self.P.on(eng, lambda e: e.tensor_tensor(out=o, in0=a, in1=b, op=op), reads=[A, B], writes=[O])

    def ts(self, eng, O, o, A, a, s1, s2, op0, op1=None, extra=()):
        if op1 is None:
            self.P.on(eng, lambda e: e.tensor_scalar(out=o, in0=a, scalar1=s1, scalar2=None, op0=op0),
                      reads=[A] + list(extra), writes=[O])
        else:
            self.P.on(eng, lambda e: e.tensor_scalar(out=o, in0=a, scalar1=s1, scalar2=s2, op0=op0, op1=op1),
                      reads=[A] + list(extra), writes=[O])

    def stt(self, eng, O, o, A, a, s, B, b, op0, op1, extra=()):
        self.P.on(eng, lambda e: e.scalar_tensor_tensor(out=o, in0=a, scalar=s, in1=b, op0=op0, op1=op1),
                  reads=[A, B] + list(extra), writes=[O])

    def memset(self, eng, O, o, val):
        self.P.on(eng, lambda e: e.memset(o, val), reads=[], writes=[O])


INPUT_NAMES = ["ada_w", "ret_w_in", "ret_w_out", "ml_w_in", "ml_w_out", "rk_w_rkvg", "rk_w_out"]


def build_program(layers=(0, 1, 2, 3), dbg=False):
    nc = bass.Bass("TRN2", target_bir_lowering=False)
    dt_in = lambda name, shape, dt=F32: nc.dram_tensor(name, list(shape), dt, kind="ExternalInput").ap()
    dt_sc = lambda name, shape, dt=F32: nc.dram_tensor(name, list(shape), dt, kind="Internal").ap()

    xin = dt_in("xin", [NT, D])
    c_pk = dt_in("c_pk", [128, 16])
    ada_w = dt_in("ada_w", [DEPTH, D, 3 * D])
    ada_b_pf = dt_in("ada_b_pf", [DEPTH, 128, 24])
    ada_b = dt_in("ada_b", [DEPTH, 3 * D])
    ln_g = dt_in("ln_g", [DEPTH, D])
    ln_b = dt_in("ln_b", [DEPTH, D])
    ret_w_in = dt_in("ret_w_in", [2, D, 6144])
    ret_decay = dt_in("ret_decay", [2, 8])
    ret_w_out = dt_in("ret_w_out", [2, 2048, D])
    ml_w_in = dt_in("ml_w_in", [1, D, 4096])
    ml_cw_pf = dt_in("ml_cw_pf", [128, 48])
    ml_cbs_pf = dt_in("ml_cbs_pf", [128, 32])
    ml_wbd = dt_in("ml_wbd", [3, 16, 128, 128])
    ml_gw_pt = dt_in("ml_gw_pt", [128, 48 * 16])
    ml_gate_b = dt_in("ml_gate_b", [16])
    ml_gn_g = dt_in("ml_gn_g", [1, 2048])
    ml_w_out = dt_in("ml_w_out", [1, 2048, D])
    rk_mix_pk = dt_in("rk_mix_pk", [128, 48])
    rk_w_rkvg = dt_in("rk_w_rkvg", [4, D, D])
    rk_w0 = dt_in("rk_w0", [2, D])
    rk_w1 = dt_in("rk_w1", [2, D, 64])
    rk_w2 = dt_in("rk_w2", [2, 64, D])
    rk_a0 = dt_in("rk_a0", [2, D])
    rk_a1 = dt_in("rk_a1", [2, D, 64])
    rk_a2 = dt_in("rk_a2", [2, 64, D])
    rk_k_k = dt_in("rk_k_k", [1, D])
    rk_k_a = dt_in("rk_k_a", [1, D])
    rk_r_k = dt_in("rk_r_k", [1, D])
    rk_gn_g = dt_in("rk_gn_g", [1, D])
    rk_gn_b = dt_in("rk_gn_b", [1, D])
    rk_w_out = dt_in("rk_w_out", [1, D, D])
    consts = dt_in("consts", [128, 1024])
    sel = dt_in("sel", [2, 256])
    lvl_masks = dt_in("lvl_masks", [128, 7 * 128])
    out = nc.dram_tensor("out", [SEQ, D], F32, kind="ExternalOutput").ap()
    dbg_xc = nc.dram_tensor("dbg_xc", [CTX, D], F32, kind="ExternalOutput").ap() if dbg else None

    xs_d = [dt_sc("xs0", [NT, D]), dt_sc("xs1", [NT, D])]
    qT_d = dt_sc("qT_d", [8, 128, NT], BF16)
    kT_d = dt_sc("kT_d", [8, 128, NT], BF16)
    v_d = dt_sc("v_d", [NT, 2048], BF16)
    g_d = dt_sc("g_d", [NT, 2048], BF16)
    of_d = dt_sc("of_d", [NT, 2048], F32)
    rk_d = dt_sc("rk_d", [4, NT, D])
    ry_d = dt_sc("ry_d", [NT, D])
    rb_d = dt_sc("rb_d", [NT, 16])
    mq_d = dt_sc("mq_d", [16, 128, NT], BF16)
    mk_d = dt_sc("mk_d", [16, 128, NT], BF16)
    zT_d = dt_sc("zT_d", [16, 128, NT], BF16)
    skx_d = dt_sc("skx_d", [16, 128, NT], BF16)
    mkt_d = dt_sc("mkt_d", [NT, 2048], BF16)
    mvt_d = dt_sc("mvt_d", [NT, 2048], BF16)

    P = Prog(nc)
    k = K(P)
    final_stores = []

    with contextlib.ExitStack() as es:
        arena_ap = es.enter_context(nc.sbuf_tensor("arena", [128, 52100], F32))
        psum = es.enter_context(nc.psum_tensor("psum", [128, 4096], F32))
        A = Arena(arena_ap)
        bank = [T(psum[:, i * 512:(i + 1) * 512], f"bank{i}") for i in range(8)]
        for b_ in bank:
            b_.b.psum = True

        cst = A.f32("cst", 1024)
        P.dma(cst.ap, consts[:, :], writes=[cst.b])
        ident = cst.ap[:, 0:128]
        DIFF = cst.ap[:, 128:256]
        TRI = cst.ap[:, 256:384]
        TRIT = cst.ap[:, 384:512]
        ROW1 = cst.ap[:, 512:640]
        COLP = cst.ap[:, 640:641]
        cosR = cst.ap[:, 704:768]
        sinR = cst.ap[:, 768:832]
        cosC = cst.ap[:, 832:896]
        sinC = cst.ap[:, 896:960]
        identb = A.bf16("identb", 128)
        k.cp("dve", identb.b, identb.ap, cst.b, ident)
        selt = A.f32("selt", 256)
        P.dma(selt.ap[0:2, :], sel[:, :], writes=[selt.b])
        sc2 = A.f32("sc2", 16)
        P.dma(sc2.ap, c_pk[:, :], writes=[sc2.b])
        k.act(sc2.b, sc2.ap, sc2.b, sc2.ap, AF.Silu)
        base_mark = A.off

        def mod_phase(li, defer_ln=False):
            modT = A.f32(f"modT{li}", 48)
            modTv = modT.ap.rearrange("p (f c) -> p f c", c=2)
            abpf = A.f32(f"abpf{li}", 24)
            P.dma(abpf.ap, ada_b_pf[li], writes=[abpf.b])
            gate_l = A.f32(f"gl{li}", 1024)
            gate_c = A.f32(f"gc{li}", 1024)
            lng = lnb = None
            if not defer_ln:
                lng = A.f32(f"lng{li}", 1024)
                lnb = A.f32(f"lnb{li}", 1024)
                P.dma(lng.ap, ln_g[li, :].partition_broadcast(128), writes=[lng.b])
                P.dma(lnb.ap, ln_b[li, :].partition_broadcast(128), writes=[lnb.b])
            mark = A.off
            grow = A.f32(f"grow{li}", 1024)
            gbrow = A.f32(f"gbrow{li}", 1024)
            P.dma(gbrow.ap[0:2, :], ada_b[li, 2048:3072].partition_broadcast(2), writes=[gbrow.b])
            wst = [A.f32(f"adaw{i}", 8 * 512) for i in range(2)]
            sc2v = sc2.ap.rearrange("p (c k) -> p k c", c=2)
            for g in range(6):
                w = wst[g % 2]
                wv = w.ap.rearrange("p (k n) -> p k n", k=8)
                P.dma(wv, ada_w[li].rearrange("(k p) n -> p k n", p=128)[:, :, g * 512:(g + 1) * 512],
                      writes=[w.b])
                for f4 in range(4):
                    f = g * 4 + f4
                    pb = bank[f % 2]
                    for kk in range(8):
                        k.mm(pb.b, pb.ap[:, 0:2], w.b, wv[:, kk, f4 * 128:(f4 + 1) * 128], sc2.b, sc2v[:, kk, :],
                             kk == 0, kk == 7)
                    k.ts("dve", modT.b, modTv[:, f, :], pb.b, pb.ap[:, 0:2], abpf.ap[:, f:f + 1], None, ALU.add,
                         extra=[abpf.b])
                if g >= 4:
                    n = g - 4
                    pb = bank[2 + n]
                    for kk in range(8):
                        k.mm(pb.b, pb.ap[0:2, :], sc2.b, sc2v[:, kk, :], w.b, wv[:, kk, :], kk == 0, kk == 7)
                    k.tt("dve", grow.b, grow.ap[0:2, n * 512:(n + 1) * 512], pb.b, pb.ap[0:2, :], gbrow.b,
                         gbrow.ap[0:2, n * 512:(n + 1) * 512], ALU.add)
            for n in range(2):
                for which, dst in ((0, gate_l), (1, gate_c)):
                    pb = bank[4 + which]
                    k.mm(pb.b, pb.ap, selt.b, selt.ap[0:2, which * 128:(which + 1) * 128], grow.b,
                         grow.ap[0:2, n * 512:(n + 1) * 512], True, True)
                    k.cp("act", dst.b, dst.ap[:, n * 512:(n + 1) * 512], pb.b, pb.ap)
            k.ts("dve", modT.b, modTv[:, 8:16, :], modT.b, modTv[:, 8:16, :], 1.0, None, ALU.add)
            P.barrier()
            A.off = mark
            return dict(modT=modT, modTv=modTv, gate_l=gate_l, gate_c=gate_c, lng=lng, lnb=lnb)

        def ht_phase(li, m, x_src, hT, hTb):
            hTv = hT.ap.rearrange("p (k t) -> p k t", k=8)
            xst = [A.f32(f"xst{i}", 1024) for i in range(2)]
            for c in range(NCH):
                xs = xst[c % 2]
                P.dma(xs.ap, x_src[c * 128:(c + 1) * 128, :], writes=[xs.b])
                col = 1 if c < 2 else 0
                for kk in range(8):
                    pb = bank[kk % 4]
                    k.tr(pb.b, pb.ap[:, 0:128], xs.b, xs.ap[:, kk * 128:(kk + 1) * 128], cst.b, ident)
                    sc = m["modTv"][:, 8 + kk, col:col + 1]
                    sh = m["modTv"][:, kk, col:col + 1]
                    if kk % 2 == 0:
                        k.act(hTb[c], hTv[:, kk, c * 128:(c + 1) * 128], pb.b, pb.ap[:, 0:128], AF.Identity,
                              bias=sh, scale=sc, extra=[m["modT"].b])
                    else:
                        k.ts("dve", hTb[c], hTv[:, kk, c * 128:(c + 1) * 128], pb.b, pb.ap[:, 0:128], sc, sh,
                             ALU.mult, ALU.add, extra=[m["modT"].b])

        def ln_out(li, m, c, ypb0, ypb1, x_src, x_dst, tmp):
            xs, t1, st, mv, rstd = tmp
            P.dma(xs.ap, x_src[c * 128:(c + 1) * 128, :], writes=[xs.b])
            gate = m["gate_c"] if c < 2 else m["gate_l"]
            for n, pb in enumerate((ypb0, ypb1)):
                k.tt("dve", t1.b, t1.ap[:, n * 512:(n + 1) * 512], pb.b, pb.ap, gate.b,
                     gate.ap[:, n * 512:(n + 1) * 512], ALU.mult)
            k.stt("dve", t1.b, t1.ap, xs.b, xs.ap, ALPHA, t1.b, t1.ap, ALU.mult, ALU.add)
            for n in range(2):
                P.dve(lambda e, n=n: e.bn_stats(out=st.ap[:, n * 6:(n + 1) * 6], in_=t1.ap[:, n * 512:(n + 1) * 512]),
                      reads=[t1.b], writes=[st.b])
            P.dve(lambda e: e.bn_aggr(out=mv.ap[:, 0:2], in_=st.ap[:, 0:12]), reads=[st.b], writes=[mv.b])
            k.act(rstd.b, rstd.ap[:, 0:1], mv.b, mv.ap[:, 1:2], AF.Sqrt, bias=m["eps"].ap[:, 0:1], scale=1.0,
                  extra=[m["eps"].b])
            P.dve(lambda e: e.reciprocal(out=rstd.ap[:, 0:1], in_=rstd.ap[:, 0:1]), reads=[rstd.b], writes=[rstd.b])
            k.ts("dve", t1.b, t1.ap, t1.b, t1.ap, mv.ap[:, 0:1], rstd.ap[:, 0:1], ALU.subtract, ALU.mult,
                 extra=[mv.b, rstd.b])
            k.tt("pool", t1.b, t1.ap, t1.b, t1.ap, m["lng"].b, m["lng"].ap, ALU.mult)
            k.tt("pool", xs.b, xs.ap, t1.b, t1.ap, m["lnb"].b, m["lnb"].ap, ALU.add)
            last = (li == DEPTH - 1)
            if last:
                if c >= 2:
                    final_stores.append(P.dma(out[(c - 2) * 128:(c - 1) * 128, :], xs.ap, reads=[xs.b], q="pool"))
            else:
                op = P.dma(x_dst[c * 128:(c + 1) * 128, :], xs.ap, reads=[xs.b], q="pool")
                if dbg:
                    final_stores.append(op)

        def retention_layer(li, j, x_src, x_dst):
            m = mod_phase(li)
            eps = A.f32("eps", 2)
            k.memset("dve", eps.b, eps.ap[:, 0:1], LN_EPS)
            k.memset("dve", eps.b, eps.ap[:, 1:2], 1e-6)
            m["eps"] = eps
            lg = A.f32("lg", 8)
            P.dma(lg.ap, ret_decay[j, :].partition_broadcast(128), writes=[lg.b])
            k.act(lg.b, lg.ap, lg.b, lg.ap, AF.Exp, scale=-1.0)
            k.act(lg.b, lg.ap, lg.b, lg.ap, AF.Ln, bias=1.0)
            k.ts("dve", lg.b, lg.ap, lg.b, lg.ap, -1.0, None, ALU.mult)
            lgn = A.f32("lgn", 8)
            k.ts("dve", lgn.b, lgn.ap, lg.b, lg.ap, -1.0, None, ALU.mult)
            maskT = A.f32("maskT", 8 * 128)
            qdec = A.f32("qdec", 8 * 128)
            kdec = A.f32("kdec", 8)
            cdec = A.f32("cdec", 8)
            tmpc = A.f32("tmpc", 128)
            for d in range(2):
                for h in range(4):
                    dh = d * 4 + h
                    sl = slice(dh * 128, (dh + 1) * 128)
                    if d == 0:
                        k.act(maskT.b, maskT.ap[:, sl], cst.b, DIFF, AF.Exp, scale=lg.ap[:, dh:dh + 1], extra=[lg.b])
                        k.stt("dve", maskT.b, maskT.ap[:, sl], maskT.b, maskT.ap[:, sl], 1.0 / 16.0, cst.b, TRI,
                              ALU.mult, ALU.mult)
                        k.act(qdec.b, qdec.ap[:, sl], cst.b, ROW1, AF.Exp, scale=lg.ap[:, dh:dh + 1], extra=[lg.b])
                        k.ts("dve", tmpc.b, tmpc.ap[:, 0:1], cst.b, COLP, -1.0, 127.0, ALU.mult, ALU.add)
                    else:
                        k.act(maskT.b, maskT.ap[:, sl], cst.b, DIFF, AF.Exp, scale=lgn.ap[:, dh:dh + 1], extra=[lgn.b])
                        k.stt("dve", maskT.b, maskT.ap[:, sl], maskT.b, maskT.ap[:, sl], 1.0 / 16.0, cst.b, TRIT,
                              ALU.mult, ALU.mult)
                        k.ts("dve", tmpc.b, tmpc.ap, cst.b, ROW1, -1.0, 129.0, ALU.mult, ALU.add)
                        k.act(qdec.b, qdec.ap[:, sl], tmpc.b, tmpc.ap, AF.Exp, scale=lg.ap[:, dh:dh + 1], extra=[lg.b])
                        k.cp("dve", tmpc.b, tmpc.ap[:, 0:1], cst.b, COLP)
                    k.act(kdec.b, kdec.ap[:, dh:dh + 1], tmpc.b, tmpc.ap[:, 0:1], AF.Exp, scale=lg.ap[:, dh:dh + 1],
                          extra=[lg.b])
                    k.ts("dve", kdec.b, kdec.ap[:, dh:dh + 1], kdec.b, kdec.ap[:, dh:dh + 1], 1.0 / 16.0, None, ALU.mult)
            k.act(cdec.b, cdec.ap, lg.b, lg.ap, AF.Exp, scale=128.0)
            lmark = A.off

            hT = A.bf16("hT", 8 * NT)
            hTb = [Buf(f"hT{c}") for c in range(NCH)]
            hTv = hT.ap.rearrange("p (k t) -> p k t", k=8)
            pmark = A.off
            ht_phase(li, m, x_src, hT, hTb)
            P.barrier()
            A.off = pmark

            wst = [A.f32(f"wst{i}", 8 * 512) for i in range(2)]
            wbf = [A.bf16(f"wbf{i}", 8 * 512) for i in range(2)]
            wsw = [A.bf16(f"wsw{i}", 8 * 512) for i in range(2)]
            r1 = [A.f32(f"r1_{i}", 512) for i in range(2)]
            r2 = [A.f32(f"r2_{i}", 512) for i in range(2)]
            ob = [A.bf16(f"ob{i}", 512) for i in range(3)]
            obi = 0
            tgroups = [(0, 256)] + [(256 + 512 * i, 512) for i in range(8)]
            win = ret_w_in[j].rearrange("(k p) n -> p k n", p=128)
            for g in range(12):
                w = wst[g % 2]
                wb_ = wbf[g % 2]
                ws_ = wsw[g % 2]
                wv = w.ap.rearrange("p (k n) -> p k n", k=8)
                wbv = wb_.ap.rearrange("p (k n) -> p k n", k=8)
                wsv = ws_.ap.rearrange("p (k n) -> p k n", k=8)
                P.dma(wv, win[:, :, g * 512:(g + 1) * 512], writes=[w.b])
                k.cp("pool", wb_.b, wbv, w.b, wv)
                if g < 4:
                    w5 = w.ap.rearrange("p (k t h e) -> p k t h e", k=8, t=4, h=2)
                    s5 = ws_.ap.rearrange("p (k t h e) -> p k t h e", k=8, t=4, h=2)
                    for hh in range(2):
                        for kk2 in range(2):
                            k.cp("pool", ws_.b, s5[:, kk2 * 4:(kk2 + 1) * 4, :, hh, :], w.b,
                                 w5[:, kk2 * 4:(kk2 + 1) * 4, :, 1 - hh, :])
                    dstT = qT_d if g < 2 else kT_d
                    for f4 in range(4):
                        ft = (g % 2) * 4 + f4
                        rowtile = (ft % 2 == 0)
                        for gi, (t0, n) in enumerate(tgroups):
                            pa = bank[(gi % 2) * 2]
                            pbk = bank[(gi % 2) * 2 + 1]
                            hb = [hTb[c] for c in range(t0 // 128, (t0 + n) // 128)]
                            for kk in range(8):
                                k.mm(pa.b, pa.ap[:, 0:n], [wb_.b] + hb[1:], wbv[:, kk, f4 * 128:(f4 + 1) * 128], hb[0],
                                     hTv[:, kk, t0:t0 + n], kk == 0, kk == 7)
                            o = ob[obi % 3]
                            obi += 1
                            if gi == 0:
                                k.cp("act", o.b, o.ap[:, 0:n], pa.b, pa.ap[:, 0:n])
                            else:
                                for kk in range(8):
                                    k.mm(pbk.b, pbk.ap[:, 0:n], [ws_.b] + hb[1:], wsv[:, kk, f4 * 128:(f4 + 1) * 128], hb[0],
                                         hTv[:, kk, t0:t0 + n], kk == 0, kk == 7)
                                tl = (t0 - 256)
                                if rowtile:
                                    r0 = tl // 64
                                    cv = cosR[:, r0:r0 + 8].unsqueeze(2).to_broadcast([128, 8, 64])
                                    sv = sinR[:, r0:r0 + 8].unsqueeze(2).to_broadcast([128, 8, 64])
                                else:
                                    cv = cosC.unsqueeze(1).to_broadcast([128, 8, 64])
                                    sv = sinC.unsqueeze(1).to_broadcast([128, 8, 64])
                                a1 = r1[gi % 2]
                                a2 = r2[gi % 2]
                                v3 = lambda ap: ap.rearrange("p (r w) -> p r w", w=64)
                                k.tt("dve", a1.b, v3(a1.ap), pa.b, v3(pa.ap), cst.b, cv, ALU.mult)
                                k.tt("dve", a2.b, v3(a2.ap), pbk.b, v3(pbk.ap), cst.b, sv, ALU.mult)
                                k.tt("pool", o.b, o.ap, a1.b, a1.ap, a2.b, a2.ap, ALU.add)
                            P.dma(dstT[ft, :, t0:t0 + n], o.ap[:, 0:n], reads=[o.b], writes=[], q="pool")
                else:
                    dst = v_d if g < 8 else g_d
                    cols = ((g - 4) % 4) * 512
                    for c in range(NCH):
                        pa = bank[4 + c % 4]
                        for kk in range(8):
                            k.mm(pa.b, pa.ap, hTb[c], hTv[:, kk, c * 128:(c + 1) * 128], wb_.b, wbv[:, kk, :],
                                 kk == 0, kk == 7)
                        o = ob[obi % 3]
                        obi += 1
                        if g < 8:
                            if c % 2 == 0:
                                k.cp("act", o.b, o.ap, pa.b, pa.ap)
                            else:
                                k.cp("dve", o.b, o.ap, pa.b, pa.ap)
                        else:
                            k.act(o.b, o.ap, pa.b, pa.ap, AF.Silu)
                        P.dma(dst[c * 128:(c + 1) * 128, cols:cols + 512], o.ap, reads=[o.b], q="pool")
            P.barrier()
            A.off = lmark

            wo32 = A.f32("wo32", 2048)
            wo = A.bf16("wo", 16 * 1024)
            wov = wo.ap.rearrange("p (k n) -> p k n", k=16)
            wosrc = ret_w_out[j].rearrange("(k p) n -> p k n", p=128)
            for kk2 in range(8):
                P.dma(wo32.ap.rearrange("p (k n) -> p k n", k=2), wosrc[:, kk2 * 2:(kk2 + 1) * 2, :], writes=[wo32.b])
                k.cp("pool", wo.b, wov[:, kk2 * 2:(kk2 + 1) * 2, :], wo32.b, wo32.ap.rearrange("p (k n) -> p k n", k=2))
            S32 = [T(None, f"S32_{i}") for i in range(8)]
            s32 = A.f32("S32", 8 * 512)
            sbf = A.bf16("Sbf", 8 * 512)
            Sbf = [Buf(f"Sbf{i}") for i in range(8)]
            s32v = s32.ap.rearrange("p (a n) -> p a n", a=8)
            sbfv = sbf.ap.rearrange("p (a n) -> p a n", a=8)
            qc = [A.bf16(f"qc{i}", 1024) for i in range(2)]
            kc = [A.bf16(f"kc{i}", 1024) for i in range(2)]
            vc = [A.bf16(f"vc{i}", 2048) for i in range(2)]
            gc = [A.bf16(f"gc_{i}", 2048) for i in range(2)]
            ofl = [A.f32(f"ofl{i}", 2048) for i in range(2)]
            osm = [A.f32(f"osm{i}", 2048) for i in range(2)]
            ks = [A.bf16(f"ks{i}", 256) for i in range(2)]
            sTm = [A.bf16(f"sTm{i}", 128) for i in range(2)]
            qs = [A.bf16(f"qs{i}", 256) for i in range(2)]
            ub = [A.bf16(f"ub{i}", 2048) for i in range(2)]
            uT = [A.bf16(f"uT{i}", 2048) for i in range(2)]
            ss = A.f32("ss", 8)
            junk = A.f32("junk", 512)
            lnt = [(A.f32(f"lnxs{i}", 1024), A.f32(f"lnt1{i}", 1024), A.f32(f"lnst{i}", 12), A.f32(f"lnmv{i}", 2),
                    A.f32(f"lnrs{i}", 2)) for i in range(2)]
            bankbf = [T(bank[i].ap.bitcast(BF16), None) for i in range(8)]
            ofb = [Buf(f"ofd{c}") for c in range(NCH)]
            for d in range(2):
                for a in range(8):
                    k.memset("dve", S32[a].b, s32v[:, a, :], 0.0)
                    k.memset("pool", Sbf[a], sbfv[:, a, :], 0.0)
                order = list(range(NCH)) if d == 0 else [1, 0] + list(range(NCH - 1, 1, -1))
                for it, c in enumerate(order):
                    q_ = qc[it % 2]
                    k_ = kc[it % 2]
                    v_ = vc[it % 2]
                    tsl = slice(c * 128, (c + 1) * 128)
                    P.dma(q_.ap.rearrange("p (f t) -> p f t", f=8), qT_d[:, :, tsl].rearrange("f p t -> p f t"),
                          writes=[q_.b])
                    P.dma(k_.ap.rearrange("p (f t) -> p f t", f=8), kT_d[:, :, tsl].rearrange("f p t -> p f t"),
                          writes=[k_.b])
                    P.dma(v_.ap, v_d[tsl, :], writes=[v_.b])
                    qv = q_.ap.rearrange("p (f t) -> p f t", f=8)
                    kv = k_.ap.rearrange("p (f t) -> p f t", f=8)
                    if d == 1:
                        ofx = ofl[it % 2]
                        P.dma(ofx.ap, of_d[tsl, :], reads=[ofb[c]], writes=[ofx.b])
                        g_ = gc[it % 2]
                        P.dma(g_.ap, g_d[tsl, :], writes=[g_.b])
                    o_ = osm[it % 2]
                    for h in range(4):
                        dh = d * 4 + h
                        hi = it * 4 + h
                        kst = ks[hi % 2]
                        pkb = bank[0]
                        for dt_ in range(2):
                            k.tr(pkb.b, bankbf[0].ap[:, dt_ * 128:(dt_ + 1) * 128], k_.b, kv[:, 2 * h + dt_, :],
                                 identb.b, identb.ap)
                        k.act(kst.b, kst.ap, pkb.b, bankbf[0].ap[:, 0:256], AF.Identity, scale=kdec.ap[:, dh:dh + 1],
                              extra=[kdec.b])
                        psb = bank[1]
                        for dt_ in range(2):
                            k.mm(psb.b, psb.ap[:, 0:128], k_.b, kv[:, 2 * h + dt_, :], q_.b, qv[:, 2 * h + dt_, :],
                                 dt_ == 0, dt_ == 1)
                        sm = sTm[hi % 2]
                        k.tt("dve", sm.b, sm.ap, psb.b, psb.ap[:, 0:128], maskT.b, maskT.ap[:, dh * 128:(dh + 1) * 128],
                             ALU.mult)
                        qs_ = qs[hi % 2]
                        k.tt("pool", qs_.b, qs_.ap.rearrange("p (a t) -> p a t", a=2), q_.b, qv[:, 2 * h:2 * h + 2, :],
                             qdec.b, qdec.ap[:, dh * 128:(dh + 1) * 128].unsqueeze(1).to_broadcast([128, 2, 128]),
                             ALU.mult)
                        pob = bank[2 + hi % 2]
                        k.mm(pob.b, pob.ap, sm.b, sm.ap, v_.b, v_.ap[:, h * 512:(h + 1) * 512], True, False)
                        for dt_ in range(2):
                            k.mm(pob.b, pob.ap, qs_.b, qs_.ap[:, dt_ * 128:(dt_ + 1) * 128], Sbf[2 * h + dt_],
                                 sbfv[:, 2 * h + dt_, :], False, dt_ == 1)
                        if d == 0:
                            k.cp("act", o_.b, o_.ap[:, h * 512:(h + 1) * 512], pob.b, pob.ap)
                        else:
                            k.tt("dve", o_.b, o_.ap[:, h * 512:(h + 1) * 512], pob.b, pob.ap, ofx.b,
                                 ofx.ap[:, h * 512:(h + 1) * 512], ALU.add)
                        for dt_ in range(2):
                            a = 2 * h + dt_
                            pdb = bank[4 + dt_]
                            k.mm(pdb.b, pdb.ap, kst.b, kst.ap[:, dt_ * 128:(dt_ + 1) * 128], v_.b,
                                 v_.ap[:, h * 512:(h + 1) * 512], True, True)
                            k.stt("dve", S32[a].b, s32v[:, a, :], S32[a].b, s32v[:, a, :], cdec.ap[:, dh:dh + 1],
                                  pdb.b, pdb.ap, ALU.mult, ALU.add, extra=[cdec.b])
                            k.cp("act", Sbf[a], sbfv[:, a, :], S32[a].b, s32v[:, a, :])
                    if d == 0:
                        P.dma(of_d[tsl, :], o_.ap, reads=[o_.b], writes=[ofb[c]], q="pool")
                    else:
                        for h in range(4):
                            k.act(junk.b, junk.ap, o_.b, o_.ap[:, h * 512:(h + 1) * 512], AF.Square,
                                  accum=ss.ap[:, h:h + 1], wextra=[ss.b])
                        k.act(ss.b, ss.ap[:, 4:8], ss.b, ss.ap[:, 0:4], AF.Sqrt, bias=eps.ap[:, 1:2], scale=1.0 / 512.0,
                              extra=[eps.b])
                        P.dve(lambda e: e.reciprocal(out=ss.ap[:, 4:8], in_=ss.ap[:, 4:8]), reads=[ss.b], writes=[ss.b])
                        u_ = ub[it % 2]
                        for h in range(4):
                            k.stt("dve", u_.b, u_.ap[:, h * 512:(h + 1) * 512], o_.b,
                                  o_.ap[:, h * 512:(h + 1) * 512], ss.ap[:, 4 + h:5 + h], g_.b,
                                  g_.ap[:, h * 512:(h + 1) * 512], ALU.mult, ALU.mult, extra=[ss.b])
                        ut = uT[it % 2]
                        utv = ut.ap.rearrange("p (k t) -> p k t", k=16)
                        for half in range(2):
                            ptb = bank[half]
                            for i8 in range(8):
                                kk = half * 8 + i8
                                k.tr(ptb.b, bankbf[half].ap[:, i8 * 128:(i8 + 1) * 128], u_.b,
                                     u_.ap[:, kk * 128:(kk + 1) * 128], identb.b, identb.ap)
                            k.cp("act", ut.b, ut.ap[:, half * 1024:(half + 1) * 1024], ptb.b, bankbf[half].ap[:, 0:1024])
                        for n in range(2):
                            pyb = bank[6 + n]
                            for kk in range(16):
                                k.mm(pyb.b, pyb.ap, ut.b, utv[:, kk, :], wo.b, wov[:, kk, n * 512:(n + 1) * 512],
                                     kk == 0, kk == 15)
                        ln_out(li, m, c, bank[6], bank[7], x_src, x_dst, lnt[it % 2])
            P.barrier()
            A.off = base_mark

        def mlstm_layer(li, j, x_src, x_dst):
            m = mod_phase(li)
            eps = A.f32("eps", 2)
            k.memset("dve", eps.b, eps.ap[:, 0:1], LN_EPS)
            m["eps"] = eps
            cw = A.f32("cw", 48)
            P.dma(cw.ap, ml_cw_pf[:, :], writes=[cw.b])
            cwv = cw.ap.rearrange("p (t j) -> p t j", j=3)
            cbs = A.f32("cbs", 32)
            P.dma(cbs.ap, ml_cbs_pf[:, :], writes=[cbs.b])
            gbb = A.f32("gbb", 16)
            P.dma(gbb.ap, ml_gate_b.partition_broadcast(128), writes=[gbb.b])
            onesf = A.f32("onesf", 128)
            k.memset("dve", onesf.b, onesf.ap, 1.0)
            onesb = A.bf16("onesb", 2)
            k.memset("dve", onesb.b, onesb.ap, 1.0)
            negf = A.f32("negf", 128)
            negb = A.f32("negb", 128)
            k.ts("dve", negf.b, negf.ap, cst.b, TRIT, -1.0, 1e30, ALU.add, ALU.mult)
            k.ts("dve", negb.b, negb.ap, cst.b, TRI, -1.0, 1e30, ALU.add, ALU.mult)
            NA = NCH * 8
            gacc = A.f32("gacc", NCH * 16)
            gav = gacc.ap.rearrange("p (c e) -> p c e", e=16)
            lmark = A.off

            hT = A.bf16("hT", 8 * NT)
            hTb = [Buf(f"hT{c}") for c in range(NCH)]
            hTv = hT.ap.rearrange("p (k t) -> p k t", k=8)
            pmark = A.off
            ht_phase(li, m, x_src, hT, hTb)
            P.barrier()
            A.off = pmark

            wbd32 = A.f32("wbd32", 128)
            wbd = A.bf16("wbd", 3 * 16 * 128)
            wbdv = wbd.ap.rearrange("p (s t f) -> p s t f", s=3, t=16)
            for s_ in range(3):
                for t in range(16):
                    P.dma(wbd32.ap, ml_wbd[s_, t], writes=[wbd32.b])
                    k.cp("pool", wbd.b, wbdv[:, s_, t, :], wbd32.b, wbd32.ap)
            gw = A.bf16("gw", 48 * 16)
            gwv = gw.ap.rearrange("p (t e) -> p t e", e=16)
            wx32 = [A.f32(f"wx32_{i}", 2048) for i in range(1)]
            wxb = [A.bf16(f"wxb_{i}", 2048) for i in range(2)]
            gw32 = wx32[0]
            P.dma(gw32.ap[:, 0:768], ml_gw_pt[:, :], writes=[gw32.b])
            k.cp("pool", gw.b, gw.ap, gw32.b, gw32.ap[:, 0:768])
            xrow = A.f32("xrow", NT)
            xcb = A.bf16("xcb", NT)
            xmb = A.bf16("xmb", NT)
            qrow = A.bf16("qrow", NT)
            krow = A.bf16("krow", NT)
            vrow = A.bf16("vrow", NT)
            zt = [A.bf16(f"zt{i}", 512) for i in range(2)]
            cvt = [A.f32(f"cvt{i}", 512) for i in range(2)]
            skt = [A.bf16(f"skt{i}", 512) for i in range(2)]
            ktg = [A.bf16(f"ktg{i}", 512) for i in range(2)]
            vtg = [A.bf16(f"vtg{i}", 512) for i in range(2)]
            tgroups = [(0, 256)] + [(256 + 512 * i, 512) for i in range(8)]
            segs = [(0, 256), (256, NT)]
            win = ml_w_in[0].rearrange("(k p) n -> p k n", p=128)
            for t in range(16):
                w32 = wx32[0]
                wb_ = wxb[t % 2]
                w32v = w32.ap.rearrange("p (s k n) -> p s k n", s=2, k=8)
                wbv = wb_.ap.rearrange("p (s k n) -> p s k n", s=2, k=8)
                P.dma(w32v[:, 0], win[:, :, t * 128:(t + 1) * 128], writes=[w32.b])
                P.dma(w32v[:, 1], win[:, :, 2048 + t * 128:2048 + (t + 1) * 128], writes=[w32.b])
                k.cp("pool", wb_.b, wb_.ap, w32.b, w32.ap)
                for gi, (t0, n) in enumerate(tgroups):
                    hb = [hTb[c] for c in range(t0 // 128, (t0 + n) // 128)]
                    pa = bank[(gi % 2) * 2]
                    pz = bank[(gi % 2) * 2 + 1]
                    for kk in range(8):
                        k.mm(pa.b, pa.ap[:, 0:n], [wb_.b] + hb[1:], wbv[:, 0, kk, :], hb[0], hTv[:, kk, t0:t0 + n], kk == 0, kk == 7)
                    k.cp("act", xrow.b, xrow.ap[:, t0:t0 + n], pa.b, pa.ap[:, 0:n])
                    for kk in range(8):
                        k.mm(pz.b, pz.ap[:, 0:n], [wb_.b] + hb[1:], wbv[:, 1, kk, :], hb[0], hTv[:, kk, t0:t0 + n], kk == 0, kk == 7)
                    z_ = zt[gi % 2]
                    k.act(z_.b, z_.ap[:, 0:n], pz.b, pz.ap[:, 0:n], AF.Silu)
                    P.dma(zT_d[t, :, t0:t0 + n], z_.ap[:, 0:n], reads=[z_.b], q="pool")
                for gi, (t0, n) in enumerate(tgroups):
                    lo = 1 if t0 in (0, 256) else 0
                    hi = 1 if (t0 + n) in (256, NT) else 0
                    cv = cvt[gi % 2]
                    k.ts("dve", cv.b, cv.ap[:, 0:n], xrow.b, xrow.ap[:, t0:t0 + n], cwv[:, t, 1:2], cbs.ap[:, t:t + 1],
                         ALU.mult, ALU.add, extra=[cw.b, cbs.b])
                    k.stt("dve", cv.b, cv.ap[:, lo:n], xrow.b, xrow.ap[:, t0 - 1 + lo:t0 + n - 1], cwv[:, t, 0:1], cv.b,
                          cv.ap[:, lo:n], ALU.mult, ALU.add, extra=[cw.b])
                    k.stt("dve", cv.b, cv.ap[:, 0:n - hi], xrow.b, xrow.ap[:, t0 + 1:t0 + n + 1 - hi], cwv[:, t, 2:3], cv.b,
                          cv.ap[:, 0:n - hi], ALU.mult, ALU.add, extra=[cw.b])
                    k.act(cv.b, cv.ap[:, 0:n], cv.b, cv.ap[:, 0:n], AF.Silu)
                    k.cp("pool", xcb.b, xcb.ap[:, t0:t0 + n], cv.b, cv.ap[:, 0:n])
                    sk = skt[gi % 2]
                    k.ts("dve", sk.b, sk.ap[:, 0:n], cv.b, cv.ap[:, 0:n], cbs.ap[:, 16 + t:17 + t], None, ALU.mult,
                         extra=[cbs.b])
                    P.dma(skx_d[t, :, t0:t0 + n], sk.ap[:, 0:n], reads=[sk.b], q="pool")
                k.cp("pool", xmb.b, xmb.ap, xrow.b, xrow.ap)
                for gi, (t0, n) in enumerate(tgroups):
                    for s_, (src, dstrow) in enumerate(((xcb, qrow), (xcb, krow), (xmb, vrow))):
                        pb = bank[4 + (gi * 3 + s_) % 2]
                        k.mm(pb.b, pb.ap[:, 0:n], wbd.b, wbdv[:, s_, t, :], src.b, src.ap[:, t0:t0 + n], True, True)
                        if s_ == 1:
                            k.cp("act", dstrow.b, dstrow.ap[:, t0:t0 + n], pb.b, pb.ap[:, 0:n])
                        else:
                            k.cp("dve", dstrow.b, dstrow.ap[:, t0:t0 + n], pb.b, pb.ap[:, 0:n])
                P.dma(mq_d[t], qrow.ap, reads=[qrow.b], q="pool")
                P.dma(mk_d[t], krow.ap, reads=[krow.b], q="pool")
                for c4 in range(0, NCH, 4):
                    nn = min(4, NCH - c4)
                    for s_, (src, dsttt, dd_) in ((1, (xcb, ktg, mkt_d)), (2, (xmb, vtg, mvt_d))):
                        pb = bank[6 + (s_ % 2)]
                        for i_ in range(nn):
                            c = c4 + i_
                            k.mm(pb.b, pb.ap[:, i_ * 128:(i_ + 1) * 128], src.b, src.ap[:, c * 128:(c + 1) * 128], wbd.b,
                                 wbdv[:, s_, t, :], True, True)
                        tg_ = dsttt[(c4 // 4) % 2]
                        k.cp("act" if s_ == 1 else "dve", tg_.b, tg_.ap[:, 0:nn * 128], pb.b, pb.ap[:, 0:nn * 128])
                        P.dma(dd_.rearrange("(c p) f -> p c f", p=128)[:, c4:c4 + nn, t * 128:(t + 1) * 128],
                              tg_.ap[:, 0:nn * 128].rearrange("p (c f) -> p c f", f=128), reads=[tg_.b], q="pool")
                for bi, (c0, c1) in enumerate(((0, 32), (32, NCH))):
                    pb = bank[bi]
                    for c in range(c0, c1):
                        for s_, row in enumerate((qrow, krow, vrow)):
                            k.mm(pb.b, pb.ap[:, (c - c0) * 16:(c - c0 + 1) * 16], row.b, row.ap[:, c * 128:(c + 1) * 128],
                                 gw.b, gwv[:, s_ * 16 + t, :], s_ == 0, s_ == 2)
                    w_ = (c1 - c0) * 16
                    if t == 0:
                        k.cp("dve", gacc.b, gacc.ap[:, c0 * 16:c0 * 16 + w_], pb.b, pb.ap[:, 0:w_])
                    else:
                        k.tt("dve", gacc.b, gacc.ap[:, c0 * 16:c0 * 16 + w_], pb.b, pb.ap[:, 0:w_], gacc.b,
                             gacc.ap[:, c0 * 16:c0 * 16 + w_], ALU.add)
            k.tt("dve", gacc.b, gav, gacc.b, gav, gbb.b, gbb.ap.unsqueeze(1).to_broadcast([128, NCH, 16]), ALU.add)
            P.barrier()
            A.off = lmark

            arr = {n_: A.f32(n_, NA) for n_ in ("LF", "Bc", "BL", "G", "CM", "REF", "E1", "AA", "BQ", "EN", "E2", "DEC")}
            av = {n_: t_.ap.rearrange("p (c e) -> p c e", e=8) for n_, t_ in arr.items()}
            amark = A.off
            gav4 = gacc.ap.rearrange("p (c d e) -> p c d e", d=2, e=8)
            LFv = arr["LF"].ap.rearrange("p (c d h) -> p c d h", d=2, h=4)
            k.act(arr["LF"].b, LFv, gacc.b, gav4[:, :, :, 4:8], AF.Exp, scale=-1.0)
            k.act(arr["LF"].b, arr["LF"].ap, arr["LF"].b, arr["LF"].ap, AF.Ln, bias=1.0)
            k.ts("dve", arr["LF"].b, arr["LF"].ap, arr["LF"].b, arr["LF"].ap, -1.0, None, ALU.mult)
            Bv4 = arr["Bc"].ap.rearrange("p (c d h) -> p c d h", d=2, h=4)
            for d in range(2):
                pb = bank[d]
                k.mm(pb.b, pb.ap[:, 0:NCH * 4], cst.b, TRI if d == 0 else TRIT, arr["LF"].b, LFv[:, :, d, :], True, True)
                k.cp("dve", arr["Bc"].b, Bv4[:, :, d, :], pb.b, pb.ap[:, 0:NCH * 4].rearrange("p (c h) -> p c h", h=4))
            pb = bank[2]
            k.mm(pb.b, pb.ap[:, 0:NA], onesf.b, onesf.ap, arr["LF"].b, arr["LF"].ap, True, True)
            k.cp("dve", arr["BL"].b, arr["BL"].ap, pb.b, pb.ap[:, 0:NA])
            Gv4 = arr["G"].ap.rearrange("p (c d h) -> p c d h", d=2, h=4)
            k.tt("dve", arr["G"].b, Gv4, gacc.b, gav4[:, :, :, 0:4], arr["Bc"].b, Bv4, ALU.subtract)
            dg = [A.f32(f"dg{i}", 512) for i in range(2)]
            tmx = [A.f32(f"tmx{i}", 512) for i in range(2)]
            it = 0
            for c in range(NCH):
                for d in range(2):
                    dgt = dg[it % 2]
                    tx = tmx[it % 2]
                    pb = bank[4 + it % 2]
                    it += 1
                    k.tt("dve", dgt.b, dgt.ap.rearrange("p (h f) -> p h f", h=4), cst.b,
                         ident.unsqueeze(1).to_broadcast([128, 4, 128]), arr["G"].b,
                         av["G"][:, c, d * 4:(d + 1) * 4].unsqueeze(2).to_broadcast([128, 4, 128]), ALU.mult)
                    k.mm(pb.b, pb.ap, onesf.b, onesf.ap, dgt.b, dgt.ap, True, True)
                    pv = pb.ap.rearrange("p (h f) -> p h f", h=4)
                    P.dve(lambda e, o=av["REF"][:, c, d * 4:(d + 1) * 4], i_=pv: e.tensor_reduce(
                        out=o, in_=i_, axis=mybir.AxisListType.X, op=ALU.max), reads=[pb.b], writes=[arr["REF"].b])
                    ng = negf if d == 0 else negb
                    k.tt("dve", tx.b, tx.ap.rearrange("p (h f) -> p h f", h=4), pb.b, pv, ng.b,
                         ng.ap.unsqueeze(1).to_broadcast([128, 4, 128]), ALU.add)
                    P.dve(lambda e, o=av["CM"][:, c, d * 4:(d + 1) * 4], i_=tx.ap.rearrange("p (h f) -> p h f", h=4):
                          e.tensor_reduce(out=o, in_=i_, axis=mybir.AxisListType.X, op=ALU.max),
                          reads=[tx.b], writes=[arr["CM"].b])
            k.tt("dve", arr["E1"].b, arr["E1"].ap, arr["G"].b, arr["G"].ap, arr["REF"].b, arr["REF"].ap, ALU.subtract)
            k.act(arr["E1"].b, arr["E1"].ap, arr["E1"].b, arr["E1"].ap, AF.Exp)
            mcol = A.f32("mcol", 4)
            Mt = A.f32("Mt", 4)
            Rt = A.f32("Rt", 4)
            ex = A.f32("ex", 20)
            exv = ex.ap.rearrange("p (s h) -> p s h", h=4)
            KS = 512.0 ** -0.5
            for d in range(2):
                k.memset("dve", mcol.b, mcol.ap, 0.0)
                order = list(range(NCH)) if d == 0 else [1, 0] + list(range(NCH - 1, 1, -1))
                hs = slice(d * 4, (d + 1) * 4)
                for c in order:
                    k.tt("dve", Mt.b, Mt.ap, arr["CM"].b, av["CM"][:, c, hs], mcol.b, mcol.ap, ALU.max)
                    k.tt("dve", Rt.b, Rt.ap, arr["REF"].b, av["REF"][:, c, hs], mcol.b, mcol.ap, ALU.max)
                    k.tt("dve", ex.b, exv[:, 0, :], arr["REF"].b, av["REF"][:, c, hs], Mt.b, Mt.ap, ALU.subtract)
                    k.tt("dve", ex.b, exv[:, 1, :], mcol.b, mcol.ap, Mt.b, Mt.ap, ALU.subtract)
                    k.stt("dve", ex.b, exv[:, 2, :], arr["Bc"].b, av["Bc"][:, c, hs], -1.0, Mt.b, Mt.ap, ALU.mult, ALU.subtract)
                    k.tt("dve", ex.b, exv[:, 3, :], arr["REF"].b, av["REF"][:, c, hs], Rt.b, Rt.ap, ALU.subtract)
                    k.tt("dve", ex.b, exv[:, 4, :], mcol.b, mcol.ap, Rt.b, Rt.ap, ALU.subtract)
                    k.act(ex.b, ex.ap, ex.b, ex.ap, AF.Exp)
                    k.cp("pool", arr["AA"].b, av["AA"][:, c, hs], ex.b, exv[:, 0, :])
                    k.cp("pool", arr["BQ"].b, av["BQ"][:, c, hs], ex.b, exv[:, 1, :])
                    k.cp("pool", arr["EN"].b, av["EN"][:, c, hs], ex.b, exv[:, 2, :])
                    k.stt("dve", arr["E2"].b, av["E2"][:, c, hs], arr["E1"].b, av["E1"][:, c, hs], KS, ex.b, exv[:, 3, :],
                          ALU.mult, ALU.mult)
                    k.cp("pool", arr["DEC"].b, av["DEC"][:, c, hs], ex.b, exv[:, 4, :])
                    k.tt("dve", mcol.b, mcol.ap, arr["BL"].b, av["BL"][:, c, hs], Rt.b, Rt.ap, ALU.add)
            k.ts("dve", arr["E1"].b, arr["E1"].ap, arr["E1"].b, arr["E1"].ap, KS, None, ALU.mult)
            P.barrier()

            c32 = A.f32("C32", 16 * 512)
            cbf = A.bf16("Cbf", 16 * 512)
            c32v = c32.ap.rearrange("p (a n) -> p a n", a=16)
            cbfv = cbf.ap.rearrange("p (a n) -> p a n", a=16)
            C32 = [Buf(f"C32_{i}") for i in range(16)]
            Cbf = [Buf(f"Cbf_{i}") for i in range(16)]
            n32 = A.f32("n32", 16)
            nbf = A.bf16("nbf", 16)
            qc = [A.bf16(f"qc{i}", 2048) for i in range(2)]
            kc = [A.bf16(f"kc{i}", 2048) for i in range(2)]
            ktc = [A.bf16(f"ktc{i}", 2048) for i in range(2)]
            vtc = [A.bf16(f"vtc{i}", 2048) for i in range(2)]
            hfl = [A.f32(f"hfl{i}", 2048) for i in range(2)]
            osm = [A.f32(f"osm{i}", 2048) for i in range(2)]
            wk = [A.bf16(f"wk{i}", 512) for i in range(2)]
            sTm = [A.bf16(f"sTm{i}", 128) for i in range(2)]
            tA = [A.f32(f"tA{i}", 512) for i in range(2)]
            sc_ = [A.f32(f"sc{i}", 8) for i in range(2)]
            ofb = [Buf(f"ofd{c}") for c in range(NCH)]
            for d in range(2):
                for a in range(16):
                    k.memset("dve", C32[a], c32v[:, a, :], 0.0)
                    k.memset("pool", Cbf[a], cbfv[:, a, :], 0.0)
                k.memset("dve", n32.b, n32.ap, 0.0)
                k.memset("dve", nbf.b, nbf.ap, 0.0)
                order = list(range(NCH)) if d == 0 else [1, 0] + list(range(NCH - 1, 1, -1))
                tri = TRI if d == 0 else TRIT
                for it, c in enumerate(order):
                    q_, k_, kt_, vt_ = qc[it % 2], kc[it % 2], ktc[it % 2], vtc[it % 2]
                    tsl = slice(c * 128, (c + 1) * 128)
                    qv = q_.ap.rearrange("p (f t) -> p f t", f=16)
                    kv = k_.ap.rearrange("p (f t) -> p f t", f=16)
                    P.dma(qv, mq_d[:, :, tsl].rearrange("f p t -> p f t"), writes=[q_.b])
                    P.dma(kv, mk_d[:, :, tsl].rearrange("f p t -> p f t"), writes=[k_.b])
                    P.dma(kt_.ap, mkt_d[tsl, :], writes=[kt_.b])
                    P.dma(vt_.ap, mvt_d[tsl, :], writes=[vt_.b])
                    if d == 1:
                        hfx = hfl[it % 2]
                        P.dma(hfx.ap, of_d[tsl, :], reads=[ofb[c]], writes=[hfx.b])
                    o_ = osm[it % 2]
                    for h in range(4):
                        dh = d * 4 + h
                        hi = it * 4 + h
                        psb = bank[0]
                        for dt_ in range(4):
                            k.mm(psb.b, psb.ap[:, 0:128], k_.b, kv[:, 4 * h + dt_, :], q_.b, qv[:, 4 * h + dt_, :],
                                 dt_ == 0, dt_ == 3)
                        sm = sTm[hi % 2]
                        k.stt("dve", sm.b, sm.ap, psb.b, psb.ap[:, 0:128], av["E1"][:, c, dh:dh + 1], cst.b, tri,
                              ALU.mult, ALU.mult, extra=[arr["E1"].b])
                        wk_ = wk[hi % 2]
                        k.act(wk_.b, wk_.ap, kt_.b, kt_.ap[:, h * 512:(h + 1) * 512], AF.Identity,
                              scale=av["E2"][:, c, dh:dh + 1], extra=[arr["E2"].b])
                        pia = bank[1]
                        k.mm(pia.b, pia.ap, sm.b, sm.ap, vt_.b, vt_.ap[:, h * 512:(h + 1) * 512], True, True)
                        pie = bank[2]
                        for dt_ in range(4):
                            k.mm(pie.b, pie.ap, q_.b, qv[:, 4 * h + dt_, :], Cbf[4 * h + dt_], cbfv[:, 4 * h + dt_, :],
                                 dt_ == 0, dt_ == 3)
                        pdn = bank[3]
                        k.mm(pdn.b, pdn.ap[:, 0:1], sm.b, sm.ap, onesb.b, onesb.ap[:, 0:1], True, True)
                        for dt_ in range(4):
                            k.mm(pdn.b, pdn.ap[:, 1:2], q_.b, qv[:, 4 * h + dt_, :], nbf.b,
                                 nbf.ap[:, 4 * h + dt_:4 * h + dt_ + 1], dt_ == 0, dt_ == 3)
                        s_ = sc_[hi % 2]
                        k.ts("dve", s_.b, s_.ap[:, 0:1], pdn.b, pdn.ap[:, 0:1], av["AA"][:, c, dh:dh + 1], None, ALU.mult,
                             extra=[arr["AA"].b])
                        k.stt("dve", s_.b, s_.ap[:, 1:2], pdn.b, pdn.ap[:, 1:2], av["BQ"][:, c, dh:dh + 1], s_.b,
                              s_.ap[:, 0:1], ALU.mult, ALU.add, extra=[arr["BQ"].b])
                        k.ts("dve", s_.b, s_.ap[:, 2:3], s_.b, s_.ap[:, 1:2], -1.0, None, ALU.mult)
                        k.tt("dve", s_.b, s_.ap[:, 2:3], s_.b, s_.ap[:, 2:3], s_.b, s_.ap[:, 1:2], ALU.max)
                        k.tt("dve", s_.b, s_.ap[:, 2:3], s_.b, s_.ap[:, 2:3], arr["EN"].b, av["EN"][:, c, dh:dh + 1],
                             ALU.max)
                        P.dve(lambda e, s_=s_: e.reciprocal(out=s_.ap[:, 3:4], in_=s_.ap[:, 2:3]), reads=[s_.b], writes=[s_.b])
                        k.tt("dve", s_.b, s_.ap[:, 4:5], s_.b, s_.ap[:, 3:4], arr["AA"].b, av["AA"][:, c, dh:dh + 1], ALU.mult)
                        k.tt("dve", s_.b, s_.ap[:, 5:6], s_.b, s_.ap[:, 3:4], arr["BQ"].b, av["BQ"][:, c, dh:dh + 1], ALU.mult)
                        ta = tA[hi % 2]
                        k.act(ta.b, ta.ap, pia.b, pia.ap, AF.Identity, scale=s_.ap[:, 4:5], extra=[s_.b])
                        k.stt("dve", o_.b, o_.ap[:, h * 512:(h + 1) * 512], pie.b, pie.ap, s_.ap[:, 5:6], ta.b, ta.ap,
                              ALU.mult, ALU.add, extra=[s_.b])
                        if d == 1:
                            k.tt("pool", o_.b, o_.ap[:, h * 512:(h + 1) * 512], o_.b, o_.ap[:, h * 512:(h + 1) * 512],
                                 hfx.b, hfx.ap[:, h * 512:(h + 1) * 512], ALU.add)
                        pn = bank[6]
                        for dt_ in range(4):
                            a = 4 * h + dt_
                            pdb = bank[4 + dt_ % 2]
                            k.mm(pdb.b, pdb.ap, wk_.b, wk_.ap[:, dt_ * 128:(dt_ + 1) * 128], vt_.b,
                                 vt_.ap[:, h * 512:(h + 1) * 512], True, True)
                            k.stt("dve", C32[a], c32v[:, a, :], C32[a], c32v[:, a, :], av["DEC"][:, c, dh:dh + 1], pdb.b,
                                  pdb.ap, ALU.mult, ALU.add, extra=[arr["DEC"].b])
                            k.cp("act", Cbf[a], cbfv[:, a, :], C32[a], c32v[:, a, :])
                            k.mm(pn.b, pn.ap[:, dt_:dt_ + 1], wk_.b, wk_.ap[:, dt_ * 128:(dt_ + 1) * 128], onesb.b,
                                 onesb.ap[:, 0:1], True, True)
                        k.stt("dve", n32.b, n32.ap[:, 4 * h:4 * h + 4], n32.b, n32.ap[:, 4 * h:4 * h + 4],
                              av["DEC"][:, c, dh:dh + 1], pn.b, pn.ap[:, 0:4], ALU.mult, ALU.add, extra=[arr["DEC"].b])
                        k.cp("dve", nbf.b, nbf.ap[:, 4 * h:4 * h + 4], n32.b, n32.ap[:, 4 * h:4 * h + 4])
                    P.dma(of_d[tsl, :], o_.ap, reads=[o_.b], writes=[ofb[c]], q="pool")
            P.barrier()
            A.off = lmark

            wo32 = A.f32("wo32", 2048)
            wo = A.bf16("wo", 16 * 1024)
            wov = wo.ap.rearrange("p (k n) -> p k n", k=16)
            wosrc = ml_w_out[0].rearrange("(k p) n -> p k n", p=128)
            for kk2 in range(8):
                P.dma(wo32.ap.rearrange("p (k n) -> p k n", k=2), wosrc[:, kk2 * 2:(kk2 + 1) * 2, :], writes=[wo32.b])
                k.cp("pool", wo.b, wov[:, kk2 * 2:(kk2 + 1) * 2, :], wo32.b, wo32.ap.rearrange("p (k n) -> p k n", k=2))
            gng = A.f32("gng", 2048)
            P.dma(gng.ap, ml_gn_g[0, :].partition_broadcast(128), writes=[gng.b])
            hsl = [A.f32(f"hsl{i}", 2048) for i in range(2)]
            skc = [A.bf16(f"skc{i}", 2048) for i in range(2)]
            szc = [A.bf16(f"szc{i}", 2048) for i in range(2)]
            onb = [A.bf16(f"onb{i}", 2048) for i in range(2)]
            uT = [A.bf16(f"uT{i}", 2048) for i in range(2)]
            u32 = [A.f32(f"u32{i}", 1024) for i in range(2)]
            st8 = [A.f32(f"st8{i}", 24) for i in range(2)]
            mv8 = [A.f32(f"mv8{i}", 16) for i in range(2)]
            lnt = [(A.f32(f"lnxs{i}", 1024), A.f32(f"lnt1{i}", 1024), A.f32(f"lnst{i}", 12), A.f32(f"lnmv{i}", 2),
                    A.f32(f"lnrs{i}", 2)) for i in range(2)]
            bankbf = [bank[i].ap.bitcast(BF16) for i in range(8)]
            for c in range(NCH):
                tsl = slice(c * 128, (c + 1) * 128)
                hs_ = hsl[c % 2]
                P.dma(hs_.ap, of_d[tsl, :], writes=[hs_.b])
                sk_ = skc[c % 2]
                sz_ = szc[c % 2]
                P.dma(sk_.ap.rearrange("p (f t) -> p f t", f=16), skx_d[:, :, tsl].rearrange("f p t -> p f t"), writes=[sk_.b])
                P.dma(sz_.ap.rearrange("p (f t) -> p f t", f=16), zT_d[:, :, tsl].rearrange("f p t -> p f t"), writes=[sz_.b])
                st_ = st8[c % 2]
                mv_ = mv8[c % 2]
                on_ = onb[c % 2]
                for h in range(4):
                    P.dve(lambda e, st_=st_, hs_=hs_, h=h: e.bn_stats(out=st_.ap[:, h * 6:(h + 1) * 6],
                                                                    in_=hs_.ap[:, h * 512:(h + 1) * 512]),
                          reads=[hs_.b], writes=[st_.b])
                    P.dve(lambda e, st_=st_, mv_=mv_, h=h: e.bn_aggr(out=mv_.ap[:, h * 2:(h + 1) * 2],
                                                                   in_=st_.ap[:, h * 6:(h + 1) * 6]),
                          reads=[st_.b], writes=[mv_.b])
                mvv = mv_.ap[:, 0:8].rearrange("p (h s) -> p h s", s=2)
                k.act(mv_.b, mv_.ap[:, 8:12], mv_.b, mvv[:, :, 1], AF.Sqrt, bias=eps.ap[:, 0:1], scale=1.0, extra=[eps.b])
                P.dve(lambda e, mv_=mv_: e.reciprocal(out=mv_.ap[:, 8:12], in_=mv_.ap[:, 8:12]), reads=[mv_.b], writes=[mv_.b])
                for h in range(4):
                    k.ts("dve", hs_.b, hs_.ap[:, h * 512:(h + 1) * 512], hs_.b, hs_.ap[:, h * 512:(h + 1) * 512],
                         mv_.ap[:, 2 * h:2 * h + 1], mv_.ap[:, 8 + h:9 + h], ALU.subtract, ALU.mult, extra=[mv_.b])
                k.tt("pool", on_.b, on_.ap, hs_.b, hs_.ap, gng.b, gng.ap, ALU.mult)
                ut = uT[c % 2]
                utv = ut.ap.rearrange("p (k t) -> p k t", k=16)
                for half in range(2):
                    ptb = bank[half]
                    for i8 in range(8):
                        kk = half * 8 + i8
                        k.tr(ptb.b, bankbf[half][:, i8 * 128:(i8 + 1) * 128], on_.b, on_.ap[:, kk * 128:(kk + 1) * 128],
                             identb.b, identb.ap)
                    u3 = u32[half]
                    k.tt("dve", u3.b, u3.ap, ptb.b, bankbf[half][:, 0:1024], sk_.b, sk_.ap[:, half * 1024:(half + 1) * 1024],
                         ALU.add)
                    k.tt("pool", ut.b, ut.ap[:, half * 1024:(half + 1) * 1024], u3.b, u3.ap, sz_.b,
                         sz_.ap[:, half * 1024:(half + 1) * 1024], ALU.mult)
                for n in range(2):
                    pyb = bank[6 + n]
                    for kk in range(16):
                        k.mm(pyb.b, pyb.ap, ut.b, utv[:, kk, :], wo.b, wov[:, kk, n * 512:(n + 1) * 512], kk == 0, kk == 15)
                ln_out(li, m, c, bank[6], bank[7], x_src, x_dst, lnt[c % 2])
            P.barrier()
            A.off = base_mark

        def rwkv_layer(li, j, x_src, x_dst):
            m = mod_phase(li, defer_ln=True)
            eps = A.f32("eps", 2)
            k.memset("dve", eps.b, eps.ap[:, 0:1], LN_EPS)
            k.memset("dve", eps.b, eps.ap[:, 1:2], 64e-5)
            m["eps"] = eps
            C0 = float(np.exp(-0.5))
            rr = [0]

            def nb():
                rr[0] = (rr[0] + 1) % 8
                return bank[rr[0]]

            onesf = A.f32("onesf", 128)
            k.memset("dve", onesf.b, onesf.ap, 1.0)
            msk = A.f32("msk", 256)
            k.tt("dve", msk.b, msk.ap[:, 0:128], cst.b, TRI, cst.b, ident, ALU.subtract)
            k.tt("dve", msk.b, msk.ap[:, 128:256], cst.b, TRIT, cst.b, ident, ALU.subtract)
            MSF = msk.ap[:, 0:128]
            MSB = msk.ap[:, 128:256]
            mixp = A.f32("mixp", 48)
            P.dma(mixp.ap, rk_mix_pk[:, :], writes=[mixp.b])
            mixv = mixp.ap.rearrange("p (j k) -> p j k", k=8)
            LW = A.bf16("LW", NT)
            LA = A.bf16("LA", NT)
            lmark = A.off

            hT = A.bf16("hT", 8 * NT)
            hTb = [Buf(f"hT{c}") for c in range(NCH)]
            hTv = hT.ap.rearrange("p (k t) -> p k t", k=8)
            xx = A.bf16("xxT", 8 * NT)
            xxv = xx.ap.rearrange("p (k t) -> p k t", k=8)
            pmark = A.off
            ht_phase(li, m, x_src, hT, hTb)
            P.barrier()
            A.off = pmark
            hall = Buf("hall")
            k.memset("pool", xx.b, xx.ap, 0.0)
            lat = lambda v, k0, k1: v[:, k0:k1, 256:NT].rearrange("p k (r c) -> p k r c", c=64)
            k.cp("pool", xx.b, lat(xxv, 0, 2)[:, :, :, 1:64], hall, lat(hTv, 0, 2)[:, :, :, 0:63])
            k.cp("pool", xx.b, lat(xxv, 2, 4)[:, :, :, 0:63], hall, lat(hTv, 2, 4)[:, :, :, 1:64])
            k.cp("pool", xx.b, xxv[:, 4:6, 256 + 64:NT], hall, hTv[:, 4:6, 256:NT - 64])
            k.cp("pool", xx.b, xxv[:, 6:8, 256:NT - 64], hall, hTv[:, 6:8, 256 + 64:NT])
            k.cp("pool", xx.b, xxv[:, 0:4, 1:256], hall, hTv[:, 0:4, 0:255])
            k.cp("pool", xx.b, xxv[:, 4:8, 0:255], hall, hTv[:, 4:8, 1:256])
            for kk in range(8):
                k.tt("dve" if kk % 2 == 0 else "pool", xx.b, xxv[:, kk, :], xx.b, xxv[:, kk, :], hall, hTv[:, kk, :],
                     ALU.subtract)
            xb = xx.b

            wst = A.f32("wst", 4 * 512)
            wbf = [A.bf16(f"wbf{i}", 8 * 512) for i in range(1)]
            wmx = [A.bf16(f"wmx{i}", 8 * 512) for i in range(1)]
            ob = [A.f32(f"ob{i}", 512) for i in range(2)]
            obi = 0
            wv = wst.ap.rearrange("p (k n) -> p k n", k=4)
            mixidx = [0, 2, 3, 5]
            for g in range(8):
                s_ = g // 2
                half = g % 2
                wb_ = wbf[0]
                wm_ = wmx[0]
                wbv = wb_.ap.rearrange("p (k n) -> p k n", k=8)
                wmv = wm_.ap.rearrange("p (k n) -> p k n", k=8)
                for k4 in range(2):
                    P.dma(wv, rk_w_rkvg[s_].rearrange("(k p) n -> p k n", p=128)[:, k4 * 4:(k4 + 1) * 4,
                                                                                   half * 512:(half + 1) * 512],
                          writes=[wst.b])
                    k.cp("pool", wb_.b, wbv[:, k4 * 4:(k4 + 1) * 4, :], wst.b, wv)
                    k.tt("dve", wm_.b, wmv[:, k4 * 4:(k4 + 1) * 4, :], wst.b, wv, mixp.b,
                         mixv[:, mixidx[s_], k4 * 4:(k4 + 1) * 4].unsqueeze(2).to_broadcast([128, 4, 512]), ALU.mult)
                for c in range(NCH):
                    pa = nb()
                    for kk in range(8):
                        k.mm(pa.b, pa.ap, hall, hTv[:, kk, c * 128:(c + 1) * 128], wb_.b, wbv[:, kk, :], kk == 0, False)
                    for kk in range(8):
                        k.mm(pa.b, pa.ap, xb, xxv[:, kk, c * 128:(c + 1) * 128], wm_.b, wmv[:, kk, :], False, kk == 7)
                    o = ob[obi % 2]
                    obi += 1
                    k.cp("act" if c % 2 == 0 else "dve", o.b, o.ap, pa.b, pa.ap)
                    P.dma(rk_d[s_, c * 128:(c + 1) * 128, half * 512:(half + 1) * 512], o.ap, reads=[o.b], q="pool")
            tgroups = [(0, 256)] + [(256 + 512 * i, 512) for i in range(8)]
            for which, (src, mi, dstrow) in enumerate(((rk_w1, 1, LW), (rk_a1, 4, LA))):
                w1v = wst.ap[:, 0:1024].rearrange("p (k d l) -> p k d l", k=8, d=2)
                for d in range(2):
                    P.dma(w1v[:, :, d, :], src[d].rearrange("(k p) l -> p k l", p=128), writes=[wst.b])
                wb_ = wbf[0]
                wm_ = wmx[0]
                wbv = wb_.ap[:, 0:1024].rearrange("p (k n) -> p k n", k=8)
                wmv = wm_.ap[:, 0:1024].rearrange("p (k n) -> p k n", k=8)
                w1f = wst.ap[:, 0:1024].rearrange("p (k n) -> p k n", k=8)
                k.cp("pool", wb_.b, wbv, wst.b, w1f)
                k.tt("dve", wm_.b, wmv, wst.b, w1f, mixp.b, mixv[:, mi, :].unsqueeze(2).to_broadcast([128, 8, 128]),
                     ALU.mult)
                for (t0, n) in tgroups:
                    pa = nb()
                    for kk in range(8):
                        k.mm(pa.b, pa.ap[:, 0:n], [wb_.b], wbv[:, kk, :], hall, hTv[:, kk, t0:t0 + n], kk == 0, False)
                    for kk in range(8):
                        k.mm(pa.b, pa.ap[:, 0:n], [wm_.b], wmv[:, kk, :], xb, xxv[:, kk, t0:t0 + n], False, kk == 7)
                    if which == 0:
                        k.act(dstrow.b, dstrow.ap[:, t0:t0 + n], pa.b, pa.ap[:, 0:n], AF.Tanh)
                    else:
                        k.cp("act", dstrow.b, dstrow.ap[:, t0:t0 + n], pa.b, pa.ap[:, 0:n])
            P.barrier()
            A.off = lmark

            if RK_DBG == 1:
                A.off = base_mark
                return
            bc = {}
            for nm, src in (("w0_0", rk_w0[0, :]), ("w0_1", rk_w0[1, :]), ("a0_0", rk_a0[0, :]), ("a0_1", rk_a0[1, :]),
                            ("k_k", rk_k_k[0, :]), ("k_a", rk_k_a[0, :]), ("r_k", rk_r_k[0, :])):
                t_ = A.f32(nm, 1024)
                P.dma(t_.ap, src.partition_broadcast(128), writes=[t_.b])
                bc[nm] = t_
            f = lambda nm: A.f32(nm, 1024)
            rc, kc_, vc_, sg, ad, Wt, Wi, Wp, kkt, at, bt, kt_, yo = [f(n_) for n_ in (
                "rc", "kc", "vc", "sg", "ad", "Wt", "Wi", "Wp", "kkt", "at", "bt", "kt", "yo")]
            kd = kc_
            rt = rc
            tmp1 = sg
            Y0 = Wt
            rm = A.f32("rm", 2)
            k.ts("dve", rm.b, rm.ap[:, 0:1], cst.b, COLP, 64.0, None, ALU.is_lt)
            k.ts("dve", rm.b, rm.ap[:, 1:2], cst.b, COLP, 64.0, None, ALU.is_ge)
            w2p = [A.bf16(f"w2p{i}", 1024) for i in range(2)]
            a2p = [A.bf16(f"a2p{i}", 1024) for i in range(2)]
            for src, dsts in ((rk_w2, w2p), (rk_a2, a2p)):
                P.dma(yo.ap, src.rearrange("d l n -> (d l) n"), writes=[yo.b])
                for dd_ in range(2):
                    k.ts("dve", dsts[dd_].b, dsts[dd_].ap, yo.b, yo.ap, rm.ap[:, dd_:dd_ + 1], None, ALU.mult, extra=[rm.b])
            XT = {n_: A.f32(n_ + "T", 1024) for n_ in ("at", "rt", "bt", "kt")}
            XP = {n_: (A.f32(n_ + "Tlo", 1024), A.f32(n_ + "Thi", 1024)) for n_ in ("at", "bt", "kt")}
            bd = A.f32("bd", 128)
            bdt = A.f32("bdt", 128)
            k.ts("dve", bdt.b, bdt.ap, cst.b, ROW1, 64.0, None, ALU.is_le)
            k.ts("dve", bd.b, bd.ap, bdt.b, bdt.ap, rm.ap[:, 0:1], None, ALU.mult, extra=[rm.b])
            k.ts("dve", bdt.b, bdt.ap, bdt.b, bdt.ap, -1.0, 1.0, ALU.mult, ALU.add)
            k.ts("dve", bdt.b, bdt.ap, bdt.b, bdt.ap, rm.ap[:, 1:2], None, ALU.mult, extra=[rm.b])
            k.tt("dve", bd.b, bd.ap, bd.b, bd.ap, bdt.b, bdt.ap, ALU.add)
            ZA = A.f32("ZA", 512)
            k.memset("pool", ZA.b, ZA.ap, 0.0)
            RhT = A.f32("RhT", 1024)
            GT = A.f32("GT", 1024)
            Z0 = A.f32("Z0", 1024)
            ST = A.f32("ST", 1024)
            WL = A.f32("WL", 8)
            ss = A.f32("ss", 32)
            bon = A.f32("bon", 32)
            Nq = [A.f32(f"Nq{i}", 512) for i in range(1)]
            Aq = [A.f32(f"Aq{i}", 512) for i in range(1)]
            lvm = A.f32("lvm", 7 * 128)
            P.dma(lvm.ap, lvl_masks[:, :], writes=[lvm.b])

            def alias2(host):
                r_ = []
                for i_ in range(2):
                    t_ = T(host.ap[:, i_ * 512:(i_ + 1) * 512], None)
                    t_.b = host.b
                    r_.append(t_)
                return r_
            Tq = alias2(Wp)
            Rq = alias2(Wi)
            Am_, Nm_ = alias2(kkt)
            M1_, K1_ = alias2(ad)
            NAK = A.f32("NAK", 512)
            NRB = A.f32("NRB", 512)
            NRK = A.f32("NRK", 512)
            Zq = [A.f32(f"Zq{i}", 512) for i in range(2)]
            q3 = lambda ap: ap.rearrange("p (h t) -> p h t", h=4)
            p3 = lambda ap: ap.rearrange("p (q t) -> p q t", t=128)
            s3 = lambda ap: ap.rearrange("p (q v) -> p q v", v=64)
            h3 = lambda ap: ap.rearrange("p (h c) -> p h c", c=64)
            yfb = [Buf(f"ryd{c}") for c in range(NCH)]
            try:
              for d in range(2):
                k.memset("dve", ST.b, ST.ap, 0.0)
                order = list(range(NCH)) if d == 0 else [1, 0] + list(range(NCH - 1, 1, -1))
                tri = TRI if d == 0 else TRIT
                ms_n = MSF if d == 0 else MSB
                ms_a = MSB if d == 0 else MSF
                w0b, a0b = bc[f"w0_{d}"], bc[f"a0_{d}"]
                if RK_DBG in (2, 4):
                    order = order[:1] if d == 0 else []
                for c in order:
                    tsl = slice(c * 128, (c + 1) * 128)
                    P.dma(rc.ap, rk_d[0, tsl, :], writes=[rc.b])
                    P.dma(kc_.ap, rk_d[1, tsl, :], writes=[kc_.b])
                    P.dma(vc_.ap, rk_d[2, tsl, :], writes=[vc_.b])
                    for (row, w2_, b0, dst_) in ((LW, w2p[d], w0b, sg), (LA, a2p[d], a0b, ad)):
                        for n in range(2):
                            pa = nb()
                            k.mm(pa.b, pa.ap, row.b, row.ap[:, tsl], w2_.b, w2_.ap[:, n * 512:(n + 1) * 512], True, True)
                            k.tt("dve", dst_.b, dst_.ap[:, n * 512:(n + 1) * 512], pa.b, pa.ap, b0.b,
                                 b0.ap[:, n * 512:(n + 1) * 512], ALU.add)
                        k.act(dst_.b, dst_.ap, dst_.b, dst_.ap, AF.Sigmoid)
                    ck(1)
                    for n in range(2):
                        pa = nb()
                        sl = slice(n * 512, (n + 1) * 512)
                        k.mm(pa.b, pa.ap, cst.b, tri, sg.b, sg.ap[:, sl], True, True)
                        k.act(Wt.b, Wt.ap[:, sl], pa.b, pa.ap, AF.Exp, scale=-C0)
                        k.act(Wi.b, Wi.ap[:, sl], pa.b, pa.ap, AF.Exp, scale=C0)
                        k.tt("dve", Wp.b, Wp.ap[:, sl], pa.b, pa.ap, sg.b, sg.ap[:, sl], ALU.subtract)
                    k.act(Wp.b, Wp.ap, Wp.b, Wp.ap, AF.Exp, scale=-C0)
                    pa = nb()
                    for pt in range(8):
                        k.mm(pa.b, pa.ap[:, pt:pt + 1], sg.b, sg.ap[:, pt * 128:(pt + 1) * 128], onesf.b, onesf.ap[:, 0:1],
                             True, True)
                    k.act(WL.b, WL.ap, pa.b, pa.ap[:, 0:8], AF.Exp, scale=-C0)
                    ck(2)
                    k.tt("dve", kkt.b, kkt.ap, kc_.b, kc_.ap, bc["k_k"].b, bc["k_k"].ap, ALU.mult)
                    k.tt("pool", tmp1.b, tmp1.ap, kkt.b, kkt.ap, kkt.b, kkt.ap, ALU.mult)
                    P.dve(lambda e: e.tensor_reduce(out=ss.ap[:, 0:16], in_=h3(tmp1.ap), axis=mybir.AxisListType.X,
                                                    op=ALU.add), reads=[tmp1.b], writes=[ss.b])
                    k.act(ss.b, ss.ap[:, 0:16], ss.b, ss.ap[:, 0:16], AF.Sqrt)
                    k.ts("dve", ss.b, ss.ap[:, 0:16], ss.b, ss.ap[:, 0:16], 1e-12, None, ALU.max)
                    P.dve(lambda e: e.reciprocal(out=ss.ap[:, 16:32], in_=ss.ap[:, 0:16]), reads=[ss.b], writes=[ss.b])
                    k.tt("dve", kkt.b, h3(kkt.ap), kkt.b, h3(kkt.ap), ss.b,
                         ss.ap[:, 16:32].unsqueeze(2).to_broadcast([128, 16, 64]), ALU.mult)
                    k.stt("dve", tmp1.b, tmp1.ap, ad.b, ad.ap, -1.0, bc["k_a"].b, bc["k_a"].ap, ALU.add, ALU.mult)
                    k.stt("dve", kd.b, kd.ap, tmp1.b, tmp1.ap, 1.0, kc_.b, kc_.ap, ALU.add, ALU.mult)
                    k.tt("pool", tmp1.b, tmp1.ap, rc.b, rc.ap, kd.b, kd.ap, ALU.mult)
                    k.tt("pool", tmp1.b, tmp1.ap, tmp1.b, tmp1.ap, bc["r_k"].b, bc["r_k"].ap, ALU.mult)
                    P.dve(lambda e: e.tensor_reduce(out=bon.ap[:, 0:16], in_=h3(tmp1.ap), axis=mybir.AxisListType.X,
                                                    op=ALU.add), reads=[tmp1.b], writes=[bon.b])
                    k.stt("dve", at.b, at.ap, kkt.b, kkt.ap, -1.0, Wp.b, Wp.ap, ALU.mult, ALU.mult)
                    k.tt("pool", rt.b, rt.ap, rc.b, rc.ap, Wt.b, Wt.ap, ALU.mult)
                    k.tt("pool", bt.b, bt.ap, kkt.b, kkt.ap, ad.b, ad.ap, ALU.mult)
                    k.tt("pool", bt.b, bt.ap, bt.b, bt.ap, Wi.b, Wi.ap, ALU.mult)
                    k.tt("dve", kt_.b, kt_.ap, kd.b, kd.ap, Wi.b, Wi.ap, ALU.mult)
                    ck(3)
                    for n_, src in (("at", at), ("rt", rt), ("bt", bt), ("kt", kt_)):
                        dstT = XT[n_]
                        for hb_ in range(2):
                            pa = nb()
                            for i4 in range(4):
                                pt = hb_ * 4 + i4
                                k.tr(pa.b, pa.ap[:, i4 * 128:(i4 + 1) * 128], src.b, src.ap[:, pt * 128:(pt + 1) * 128],
                                     cst.b, ident)
                            k.cp("act", dstT.b, dstT.ap[:, hb_ * 512:(hb_ + 1) * 512], pa.b, pa.ap)
                            if n_ in XP:
                                lo_, hi_ = XP[n_]
                                hsl_ = slice(hb_ * 512, (hb_ + 1) * 512)
                                k.ts("dve", lo_.b, lo_.ap[:, hsl_], dstT.b, dstT.ap[:, hsl_], rm.ap[:, 0:1], None,
                                     ALU.mult, extra=[rm.b])
                                k.ts("pool", hi_.b, hi_.ap[:, hsl_], dstT.b, dstT.ap[:, hsl_], rm.ap[:, 1:2], None,
                                     ALU.mult, extra=[rm.b])
                    aT, rT, bT, kT = p3(XT["at"].ap), p3(XT["rt"].ap), p3(XT["bt"].ap), p3(XT["kt"].ap)
                    aTb, rTb, bTb, kTb = XT["at"].b, XT["rt"].b, XT["bt"].b, XT["kt"].b

                    ck(4)

                    def fm(v, h):
                        return v[:, h // 2, :]

                    PD = {n_: (p3(XP[n_][0].ap), p3(XP[n_][1].ap)) for n_ in XP}
                    PDb = {n_: (XP[n_][0].b, XP[n_][1].b) for n_ in XP}

                    for q in range(4):
                        heads = [4 * q + i for i in range(4)]
                        def gram(dst, ln_, rT_, rTb_, mask):
                            pa = nb()
                            for i, h in enumerate(heads):
                                k.mm(pa.b, pa.ap[:, i * 128:(i + 1) * 128], PDb[ln_][h % 2], PD[ln_][h % 2][:, h // 2, :],
                                     rTb_, fm(rT_, h), True, True)
                            k.tt("dve", dst.b, q3(dst.ap), pa.b, q3(pa.ap), cst.b if mask in (TRI, TRIT) else msk.b,
                                 mask.unsqueeze(1).to_broadcast([128, 4, 128]), ALU.mult)
                        N0, A0 = Nq[0], Aq[0]
                        gram(N0, "bt", aT, aTb, ms_n)
                        gram(A0, "at", bT, bTb, ms_a)
                        gram(NAK, "kt", aT, aTb, ms_n)
                        gram(NRB, "bt", rT, rTb, tri)
                        gram(NRK, "kt", rT, rTb, tri)
                        ck(5)
                        Zc = Zq[0]
                        zc3 = Zc.ap.rearrange("p (h x c) -> p h x c", h=4, x=2)
                        k.cp("pool", Zc.b, zc3[:, :, 0, :], at.b, h3(at.ap)[:, 4 * q:4 * q + 4, :])
                        pa = nb()
                        for i, h in enumerate(heads):
                            k.mm(pa.b, pa.ap[:, i * 64:(i + 1) * 64], NAK.b, NAK.ap[:, i * 128:(i + 1) * 128], vc_.b,
                                 vc_.ap[:, h * 64:(h + 1) * 64], True, True)
                        k.cp("act", Zc.b, zc3[:, :, 1, :], pa.b, pa.ap[:, 0:256].rearrange("p (h c) -> p h c", c=64))
                        ck(6)
                        N0t, A0t = Nq[0], Aq[0]
                        idb = ident.unsqueeze(1).to_broadcast([128, 4, 128])
                        ti = 0
                        for lev in range(7):
                            mb = lvm.ap[:, lev * 128:(lev + 1) * 128].unsqueeze(1).to_broadcast([128, 4, 128])
                            k.tt("pool", Am_.b, q3(Am_.ap), A0t.b, q3(A0t.ap), lvm.b, mb, ALU.mult)
                            k.tt("pool", Nm_.b, q3(Nm_.ap), N0t.b, q3(N0t.ap), lvm.b, mb, ALU.mult)
                            Tc, Tn = Tq[ti % 2], Tq[(ti + 1) % 2]
                            Rc, Rn = Rq[ti % 2], Rq[(ti + 1) % 2]
                            ti += 1
                            if lev == 0:
                                k.tt("pool", Tn.b, q3(Tn.ap), Am_.b, q3(Am_.ap), cst.b, idb, ALU.add)
                                k.tt("pool", Rn.b, q3(Rn.ap), Nm_.b, q3(Nm_.ap), cst.b, idb, ALU.add)
                                continue
                            last = (lev == 6)
                            pa = nb()
                            for i in range(4):
                                sl = slice(i * 128, (i + 1) * 128)
                                k.mm(pa.b, pa.ap[:, sl], Am_.b, Am_.ap[:, sl], Rc.b, Rc.ap[:, sl], True, True)
                            k.cp("act", K1_.b, K1_.ap, pa.b, pa.ap)
                            if not last:
                                pa2 = nb()
                                for i in range(4):
                                    sl = slice(i * 128, (i + 1) * 128)
                                    k.mm(pa2.b, pa2.ap[:, sl], Nm_.b, Nm_.ap[:, sl], Tc.b, Tc.ap[:, sl], True, True)
                                k.cp("act", M1_.b, M1_.ap, pa2.b, pa2.ap)
                            pa = nb()
                            for i in range(4):
                                sl = slice(i * 128, (i + 1) * 128)
                                k.mm(pa.b, pa.ap[:, sl], Tc.b, Tc.ap[:, sl], K1_.b, K1_.ap[:, sl], True, True)
                            k.tt("dve", Rn.b, Rn.ap, pa.b, pa.ap, Rc.b, Rc.ap, ALU.add)
                            if not last:
                                pa2 = nb()
                                for i in range(4):
                                    sl = slice(i * 128, (i + 1) * 128)
                                    k.mm(pa2.b, pa2.ap[:, sl], Rc.b, Rc.ap[:, sl], M1_.b, M1_.ap[:, sl], True, True)
                                k.tt("dve", Tn.b, Tn.ap, pa2.b, pa2.ap, Tc.b, Tc.ap, ALU.add)
                        Rf = Rq[ti % 2]
                        pa = nb()
                        for i in range(4):
                            sl = slice(i * 128, (i + 1) * 128)
                            k.mm(pa.b, pa.ap[:, sl], Rf.b, Rf.ap[:, sl], Zq[0].b, Zq[0].ap[:, sl], True, True)
                        k.cp("act", Zq[1].b, Zq[1].ap, pa.b, pa.ap)
                        zi = 1
                        Zf = Zq[zi % 2]
                        zf3 = Zf.ap.rearrange("p (h x c) -> p h x c", h=4, x=2)
                        ck(7)
                        pa = nb()
                        for i, h in enumerate(heads):
                            k.mm(pa.b, pa.ap[:, i * 64:(i + 1) * 64], NRB.b, NRB.ap[:, i * 128:(i + 1) * 128], Zf.b,
                                 zf3[:, i, 1, :], True, False)
                            k.mm(pa.b, pa.ap[:, i * 64:(i + 1) * 64], NRK.b, NRK.ap[:, i * 128:(i + 1) * 128], vc_.b,
                                 vc_.ap[:, h * 64:(h + 1) * 64], False, True)
                        k.cp("act", Y0.b, Y0.ap[:, q * 256:(q + 1) * 256], pa.b, pa.ap[:, 0:256])
                        ck(8)
                        za4 = ZA.ap.rearrange("p (q e x c) -> p q e x c", q=2, e=2, x=2)
                        zf5 = Zf.ap.rearrange("p (q e x c) -> p q e x c", q=2, e=2, x=2)
                        k.cp("pool", ZA.b, za4[:, :, 0, 0, :], Zf.b, zf5[:, :, 0, 0, :])
                        k.cp("pool", ZA.b, za4[:, :, 1, 1, :], Zf.b, zf5[:, :, 1, 0, :])
                        pa = nb()
                        for i, h in enumerate(heads):
                            k.mm(pa.b, pa.ap[:, (i // 2) * 128:(i // 2 + 1) * 128], ZA.b, ZA.ap[:, i * 128:(i + 1) * 128],
                                 NRB.b, NRB.ap[:, i * 128:(i + 1) * 128], i % 2 == 0, i % 2 == 1)
                        k.tt("dve", RhT.b, RhT.ap[:, q * 256:(q + 1) * 256], pa.b, pa.ap[:, 0:256], rTb,
                             XT["rt"].ap[:, q * 256:(q + 1) * 256], ALU.add)
                        ck(9)
                        pa = nb()
                        for i, h in enumerate(heads):
                            pr = i // 2
                            k.mm(pa.b, pa.ap[:, pr * 128:(pr + 1) * 128], ZA.b, ZA.ap[:, i * 128:(i + 1) * 128], bt.b,
                                 bt.ap[:, (2 * q + pr) * 128:(2 * q + pr + 1) * 128], i % 2 == 0, i % 2 == 1)
                        for pr in range(2):
                            pt = 2 * q + pr
                            u0 = Zf.ap.rearrange("p (q e x c) -> p q e x c", q=2, e=2, x=2)[:, pr, :, 1, :]
                            k.mm(pa.b, pa.ap[:, 256 + pr * 128:256 + (pr + 1) * 128], bt.b, bt.ap[:, pt * 128:(pt + 1) * 128],
                                 Zf.b, u0, True, False)
                            k.mm(pa.b, pa.ap[:, 256 + pr * 128:256 + (pr + 1) * 128], kt_.b, kt_.ap[:, pt * 128:(pt + 1) * 128],
                                 vc_.b, vc_.ap[:, pt * 128:(pt + 1) * 128], False, True)
                        k.tt("dve", GT.b, p3(GT.ap)[:, 2 * q:2 * q + 2, :], pa.b, p3(pa.ap)[:, 0:2, :], bd.b,
                             bd.ap.unsqueeze(1).to_broadcast([128, 2, 128]), ALU.mult)
                        k.tt("dve", Z0.b, p3(Z0.ap)[:, 2 * q:2 * q + 2, :], pa.b, p3(pa.ap)[:, 2:4, :], bd.b,
                             bd.ap.unsqueeze(1).to_broadcast([128, 2, 128]), ALU.mult)
                    ck(11)
                    rh3 = p3(RhT.ap)
                    st3 = p3(ST.ap)
                    gt3 = p3(GT.ap)
                    pys = [nb(), nb()]
                    for pt in range(8):
                        k.mm(pys[pt // 4].b, pys[pt // 4].ap[:, (pt % 4) * 128:(pt % 4 + 1) * 128], RhT.b, rh3[:, pt, :],
                             ST.b, st3[:, pt, :], True, True)
                    pgs = [nb(), nb()]
                    for pt in range(8):
                        k.mm(pgs[pt // 4].b, pgs[pt // 4].ap[:, (pt % 4) * 128:(pt % 4 + 1) * 128], GT.b, gt3[:, pt, :],
                             ST.b, st3[:, pt, :], True, True)
                    for n in range(2):
                        k.tt("dve", yo.b, yo.ap[:, n * 512:(n + 1) * 512], pys[n].b, pys[n].ap, Y0.b,
                             Y0.ap[:, n * 512:(n + 1) * 512], ALU.add)
                    for n in range(2):
                        k.tt("dve", ST.b, ST.ap[:, n * 512:(n + 1) * 512], ST.b, ST.ap[:, n * 512:(n + 1) * 512], pgs[n].b,
                             pgs[n].ap, ALU.add)
                    k.tt("dve", ST.b, ST.ap, ST.b, ST.ap, Z0.b, Z0.ap, ALU.add)
                    k.tt("dve", ST.b, st3, ST.b, st3, WL.b, WL.ap.unsqueeze(2).to_broadcast([128, 8, 128]), ALU.mult)
                    ck(12)
                    if d == 1:
                        P.dma(tmp1.ap, ry_d[tsl, :], reads=[yfb[c]], writes=[tmp1.b])
                        P.dma(bon.ap[:, 16:32], rb_d[tsl, :], reads=[yfb[c]], writes=[bon.b])
                        k.tt("dve", yo.b, yo.ap, yo.b, yo.ap, tmp1.b, tmp1.ap, ALU.add)
                        k.tt("dve", bon.b, bon.ap[:, 0:16], bon.b, bon.ap[:, 0:16], bon.b, bon.ap[:, 16:32], ALU.add)
                    P.dma(ry_d[tsl, :], yo.ap, reads=[yo.b], writes=[yfb[c]], q="pool")
                    P.dma(rb_d[tsl, :], bon.ap[:, 0:16], reads=[bon.b], writes=[yfb[c]], q="pool")
            except StopBuild:
                A.off = base_mark
                return
            P.barrier()
            A.off = lmark

            if RK_DBG in (2, 3):
                A.off = base_mark
                return
            wo32 = A.f32("wo32", 2048)
            wo = A.bf16("wo", 8 * 1024)
            wov = wo.ap.rearrange("p (k n) -> p k n", k=8)
            wosrc = rk_w_out[0].rearrange("(k p) n -> p k n", p=128)
            for kk2 in range(4):
                P.dma(wo32.ap.rearrange("p (k n) -> p k n", k=2), wosrc[:, kk2 * 2:(kk2 + 1) * 2, :], writes=[wo32.b])
                k.cp("pool", wo.b, wov[:, kk2 * 2:(kk2 + 1) * 2, :], wo32.b, wo32.ap.rearrange("p (k n) -> p k n", k=2))
            m["lng"] = A.f32(f"lng{li}", 1024)
            m["lnb"] = A.f32(f"lnb{li}", 1024)
            P.dma(m["lng"].ap, ln_g[li, :].partition_broadcast(128), writes=[m["lng"].b])
            P.dma(m["lnb"].ap, ln_b[li, :].partition_broadcast(128), writes=[m["lnb"].b])
            gng = A.f32("gng", 1024)
            gnb = A.f32("gnb", 1024)
            P.dma(gng.ap, rk_gn_g[0, :].partition_broadcast(128), writes=[gng.b])
            P.dma(gnb.ap, rk_gn_b[0, :].partition_broadcast(128), writes=[gnb.b])
            ys = [A.f32(f"ys{i}", 1024) for i in range(2)]
            vs = [A.f32(f"vs{i}", 1024) for i in range(2)]
            gs = [A.f32(f"gs{i}", 1024) for i in range(2)]
            sq = [A.f32(f"sq{i}", 1024) for i in range(2)]
            bs = [A.f32(f"bs{i}", 64) for i in range(2)]
            ub = [A.bf16(f"ub{i}", 1024) for i in range(2)]
            uT = [A.bf16(f"uT{i}", 1024) for i in range(2)]
            lnt = [(A.f32(f"lnxs{i}", 1024), A.f32(f"lnt1{i}", 1024), A.f32(f"lnst{i}", 12), A.f32(f"lnmv{i}", 2),
                    A.f32(f"lnrs{i}", 2)) for i in range(2)]
            bankbf = [bank[i].ap.bitcast(BF16) for i in range(8)]
            for c in range(NCH):
                tsl = slice(c * 128, (c + 1) * 128)
                y_, v_, g_, s_, b_ = ys[c % 2], vs[c % 2], gs[c % 2], sq[c % 2], bs[c % 2]
                P.dma(y_.ap, ry_d[tsl, :], writes=[y_.b])
                P.dma(v_.ap, rk_d[2, tsl, :], writes=[v_.b])
                P.dma(g_.ap, rk_d[3, tsl, :], writes=[g_.b])
                P.dma(b_.ap[:, 0:16], rb_d[tsl, :], writes=[b_.b])
                P.dve(lambda e, b_=b_, y_=y_: e.tensor_reduce(out=b_.ap[:, 16:32], in_=h3(y_.ap), axis=mybir.AxisListType.X,
                                                            op=ALU.add), reads=[y_.b], writes=[b_.b])
                k.ts("dve", b_.b, b_.ap[:, 16:32], b_.b, b_.ap[:, 16:32], 1.0 / 64.0, None, ALU.mult)
                k.tt("dve", y_.b, h3(y_.ap), y_.b, h3(y_.ap), b_.b, b_.ap[:, 16:32].unsqueeze(2).to_broadcast([128, 16, 64]),
                     ALU.subtract)
                k.tt("pool", s_.b, s_.ap, y_.b, y_.ap, y_.b, y_.ap, ALU.mult)
                P.dve(lambda e, b_=b_, s_=s_: e.tensor_reduce(out=b_.ap[:, 32:48], in_=h3(s_.ap), axis=mybir.AxisListType.X,
                                                            op=ALU.add), reads=[s_.b], writes=[b_.b])
                k.act(b_.b, b_.ap[:, 32:48], b_.b, b_.ap[:, 32:48], AF.Sqrt, bias=eps.ap[:, 1:2], scale=1.0 / 64.0,
                      extra=[eps.b])
                P.dve(lambda e, b_=b_: e.reciprocal(out=b_.ap[:, 32:48], in_=b_.ap[:, 32:48]), reads=[b_.b], writes=[b_.b])
                k.tt("dve", y_.b, h3(y_.ap), y_.b, h3(y_.ap), b_.b, b_.ap[:, 32:48].unsqueeze(2).to_broadcast([128, 16, 64]),
                     ALU.mult)
                k.tt("pool", y_.b, y_.ap, y_.b, y_.ap, gng.b, gng.ap, ALU.mult)
                k.tt("pool", y_.b, y_.ap, y_.b, y_.ap, gnb.b, gnb.ap, ALU.add)
                k.tt("dve", s_.b, h3(s_.ap), v_.b, h3(v_.ap), b_.b, b_.ap[:, 0:16].unsqueeze(2).to_broadcast([128, 16, 64]),
                     ALU.mult)
                k.tt("pool", y_.b, y_.ap, y_.b, y_.ap, s_.b, s_.ap, ALU.add)
                k.act(g_.b, g_.ap, g_.b, g_.ap, AF.Silu)
                u_ = ub[c % 2]
                k.tt("dve", u_.b, u_.ap, y_.b, y_.ap, g_.b, g_.ap, ALU.mult)
                ut = uT[c % 2]
                utv = ut.ap.rearrange("p (k t) -> p k t", k=8)
                ptb = bank[0]
                for i8 in range(8):
                    k.tr(ptb.b, bankbf[0][:, i8 * 128:(i8 + 1) * 128], u_.b, u_.ap[:, i8 * 128:(i8 + 1) * 128], identb.b,
                         identb.ap)
                k.cp("act", ut.b, ut.ap, ptb.b, bankbf[0][:, 0:1024])
                for n in range(2):
                    pyb = bank[6 + n]
                    for kk in range(8):
                        k.mm(pyb.b, pyb.ap, ut.b, utv[:, kk, :], wo.b, wov[:, kk, n * 512:(n + 1) * 512], kk == 0, kk == 7)
                ln_out(li, m, c, bank[6], bank[7], x_src, x_dst, lnt[c % 2])
            P.barrier()
            A.off = base_mark

        cur = xin
        for li in layers:
            dst = xs_d[li % 2]
            kind, j = li % 3, li // 3
            if kind == 0:
                retention_layer(li, j, cur, dst)
            elif kind == 1:
                mlstm_layer(li, j, cur, dst)
            else:
                rwkv_layer(li, j, cur, dst)
            cur = dst
        if dbg:
            t = A.f32("dbgt", 1024)
            for c in range(2):
                P.dma(t.ap, cur[c * 128:(c + 1) * 128, :], writes=[t.b])
                final_stores.append(P.dma(dbg_xc[c * 128:(c + 1) * 128, :], t.ap, reads=[t.b]))
            if layers[-1] != DEPTH - 1:
                for c in range(2, NCH):
                    P.dma(t.ap, cur[c * 128:(c + 1) * 128, :], writes=[t.b])
                    final_stores.append(P.dma(out[(c - 2) * 128:(c - 1) * 128, :], t.ap, reads=[t.b]))
        P.emit(final_waits=final_stores)
    return nc


def host_consts():
    cst = np.zeros((128, 1024), np.float32)
    p = np.arange(128)[:, None].astype(np.float64)
    i = np.arange(128)[None, :].astype(np.float64)
    cst[:, 0:128] = np.eye(128)
    cst[:, 128:256] = i - p
    cst[:, 256:384] = (i >= p)
    cst[:, 384:512] = (i <= p)
    cst[:, 512:640] = i + 1 + 0 * p
    cst[:, 640] = p[:, 0]
    nf = 64
    inv = (10000.0 ** (-np.arange(nf, dtype=np.float32) / nf)).astype(np.float32)
    pos = np.arange(64, dtype=np.float32)
    ang = (pos[None, :] * inv[np.arange(128) % 64][:, None]).astype(np.float32)
    cos = np.cos(ang).astype(np.float32)
    sin = np.sin(ang).astype(np.float32)
    sgn = np.where(np.arange(128) < 64, -1.0, 1.0)[:, None]
    cst[:, 704:768] = cos
    cst[:, 768:832] = sin * sgn
    cst[:, 832:896] = cos
    cst[:, 896:960] = sin * sgn
    sel = np.zeros((2, 256), np.float32)
    sel[0, 0:128] = 1.0
    sel[1, 128:256] = 1.0
    return cst, sel


def _level_masks():
    p = np.arange(128)[:, None]
    i = np.arange(128)[None, :]
    out = np.zeros((128, 7 * 128), np.float32)
    for l in range(7):
        s_ = 1 << l
        same = (p // (2 * s_)) == (i // (2 * s_))
        off = ((p % (2 * s_)) >= s_) != ((i % (2 * s_)) >= s_)
        out[:, l * 128:(l + 1) * 128] = (same & off)
    return out


def _block_diag(w):
    out = np.zeros((3, 16, 128, 128), np.float32)
    for n in range(32):
        out[:, :, n * 4:(n + 1) * 4, n * 4:(n + 1) * 4] = w.reshape(3, 16, 32, 4, 4)[:, :, n]
    return out


def make_in_maps(inputs, cores):
    cst, sel = host_consts()
    shared = {
        "ada_w": np.ascontiguousarray(inputs["ada_w"]),
        "ada_b": np.ascontiguousarray(inputs["ada_b"]),
        "ada_b_pf": np.ascontiguousarray(inputs["ada_b"].reshape(DEPTH, 24, 128).transpose(0, 2, 1)),
        "ln_g": np.ascontiguousarray(inputs["ln_g"]),
        "ln_b": np.ascontiguousarray(inputs["ln_b"]),
        "ret_w_in": np.ascontiguousarray(inputs["ret_w_in"]),
        "ret_decay": np.ascontiguousarray(inputs["ret_decay"].reshape(2, 8)),
        "ret_w_out": np.ascontiguousarray(inputs["ret_w_out"]),
        "consts": cst,
        "sel": sel,
        "lvl_masks": _level_masks(),
        "rk_mix_pk": np.ascontiguousarray(inputs["rk_mix"][0].reshape(6, 8, 128).transpose(2, 0, 1).reshape(128, 48)),
        "rk_w_rkvg": np.ascontiguousarray(inputs["rk_w_rkvg"][0]),
        "rk_w0": np.ascontiguousarray(inputs["rk_w0"][0]),
        "rk_w1": np.ascontiguousarray(inputs["rk_w1"][0]),
        "rk_w2": np.ascontiguousarray(inputs["rk_w2"][0]),
        "rk_a0": np.ascontiguousarray(inputs["rk_a0"][0]),
        "rk_a1": np.ascontiguousarray(inputs["rk_a1"][0]),
        "rk_a2": np.ascontiguousarray(inputs["rk_a2"][0]),
        "rk_k_k": np.ascontiguousarray(inputs["rk_k_k"]),
        "rk_k_a": np.ascontiguousarray(inputs["rk_k_a"]),
        "rk_r_k": np.ascontiguousarray(inputs["rk_r_k"].reshape(1, 1024)),
        "rk_gn_g": np.ascontiguousarray(inputs["rk_gn_g"]),
        "rk_gn_b": np.ascontiguousarray(inputs["rk_gn_b"]),
        "rk_w_out": np.ascontiguousarray(inputs["rk_w_out"]),
        "ml_w_in": np.ascontiguousarray(inputs["ml_w_in"]),
        "ml_cw_pf": np.ascontiguousarray(inputs["ml_conv_w"][0].reshape(3, 16, 128).transpose(2, 1, 0).reshape(128, 48)),
        "ml_cbs_pf": np.ascontiguousarray(np.concatenate([inputs["ml_conv_b"][0].reshape(16, 128).T,
                                                          inputs["ml_skip"][0].reshape(16, 128).T], axis=1)),
        "ml_wbd": _block_diag(inputs["ml_w_qkv"][0]),
        "ml_gw_pt": np.ascontiguousarray(inputs["ml_gate_w"][0].reshape(2, 48, 128, 8).transpose(2, 1, 0, 3).reshape(128, 48 * 16)),
        "ml_gate_b": np.ascontiguousarray(inputs["ml_gate_b"][0].reshape(16)),
        "ml_gn_g": np.ascontiguousarray(inputs["ml_gn_g"]),
        "ml_w_out": np.ascontiguousarray(inputs["ml_w_out"]),
    }
    maps = []
    for b in cores:
        m = dict(shared)
        m["xin"] = np.ascontiguousarray(np.concatenate([inputs["ctx"][b], inputs["x"][b]], axis=0))
        cp = np.concatenate([inputs["c"][b].reshape(8, 128).T, inputs["c_ctx"].reshape(8, 128).T], axis=1)
        m["c_pk"] = np.ascontiguousarray(cp.astype(np.float32))
        maps.append(m)
    return maps


def kernel(**inputs):
    inputs = {k_: np.asarray(v) for k_, v in inputs.items()}
    nc = build_program()
    maps = make_in_maps(inputs, list(range(8)))
    res = run_bass_kernel_spmd(nc, maps, core_ids=list(range(8)))
    return np.stack([np.asarray(r["out"]) for r in res.results], axis=0).astype(np.float32)
```
